# Optimizing a Trainium2 kernel written in Bass

```python
import jax, jax.numpy as jnp
from jax import lax
import numpy as np

D_MODEL = 1024
BATCH = 16
SEQ = 4096
DEPTH = 1

A_HEADS = D_MODEL // 128
A_HEAD_DIM = 64
A_WIDTH = A_HEADS * A_HEAD_DIM
A_PATTERNS = ((128, 1), (512, 4), (2048, 16))
BLK = 128
NEG = -1e30
B_HEADS = D_MODEL // 256
B_EXPAND = 128
B_HEAD_V = 128
B_WIDTH_K = B_HEADS * B_EXPAND
B_WIDTH = B_HEADS * B_HEAD_V
B_CHUNK = 64
FFN_HIDDEN = ((8 * D_MODEL // 3 + 255) // 256) * 256
PLE_DIM = 256
EPS = 1e-6
IN_SIZES = (A_WIDTH, A_WIDTH, A_WIDTH,
            B_WIDTH_K, B_WIDTH_K, B_WIDTH, B_WIDTH,
            D_MODEL, D_MODEL)
IN_TOTAL = sum(IN_SIZES)
IN_SPLITS = [int(s) for s in np.cumsum(IN_SIZES)[:-1]]

kernel_name = "hybrid_dilated_attn_hgrn2_gated_merge"


def rmsnorm(x, g):
    xf = x.astype(jnp.float32)
    y = xf * lax.rsqrt(jnp.mean(xf * xf, axis=-1, keepdims=True) + EPS)
    return (y * g.astype(jnp.float32)).astype(x.dtype)


def _dilated_branch(q, k, v, window, dilation):
    B, H, S, Dh = q.shape
    span = window // dilation
    unit = dilation * BLK
    s_pad = -(-S // unit) * unit
    pad = s_pad - S
    L = s_pad // dilation
    nb = L // BLK

    def to_blocks(t):
        t = jnp.pad(t, ((0, 0), (0, 0), (pad, 0), (0, 0)))
        t = t.reshape(B, H, L, dilation, Dh).transpose(0, 1, 3, 2, 4)
        return t.reshape(B, H, dilation, nb, BLK, Dh)

    qb, kb, vb = to_blocks(q), to_blocks(k), to_blocks(v)

    def band(t):
        prev = jnp.pad(t[:, :, :, :-1], ((0, 0), (0, 0), (0, 0), (1, 0), (0, 0), (0, 0)))
        return jnp.concatenate([prev, t], axis=4)

    kk, vv = band(kb), band(vb)
    s = jnp.einsum("bhrnqd,bhrnkd->bhrnqk", qb, kk) * (Dh ** -0.5)
    a = jnp.arange(BLK)[:, None]
    c = jnp.arange(2 * BLK)[None, :]
    rel = a + BLK - c
    r = jnp.arange(dilation)[:, None, None, None]
    n = jnp.arange(nb)[None, :, None, None]
    key_pos = ((n - 1) * BLK + c) * dilation + r - pad
    mask = (rel >= 0) & (rel <= span) & (key_pos >= 0)
    s = jnp.where(mask, s, NEG)
    m = jnp.max(s, axis=-1)
    e = jnp.where(mask, jnp.exp(s - m[..., None]), 0.0)
    den = jnp.sum(e, axis=-1)
    num = jnp.einsum("bhrnqk,bhrnkd->bhrnqd", e, vv)

    def from_blocks(t):
        tail = t.shape[5:]
        t = t.reshape((B, H, dilation, L) + tail)
        t = jnp.swapaxes(t, 2, 3).reshape((B, H, s_pad) + tail)
        return t[:, :, pad:]

    return from_blocks(m), from_blocks(den), from_blocks(num)


def dilated_attention(qa, ka, va):
    B, S, _ = qa.shape
    heads = lambda t: t.astype(jnp.float32).reshape(B, S, A_HEADS, A_HEAD_DIM).transpose(0, 2, 1, 3)
    q, k, v = heads(qa), heads(ka), heads(va)
    ms, dens, nums = [], [], []
    for window, dilation in A_PATTERNS:
        m, den, num = _dilated_branch(q, k, v, window, dilation)
        ms.append(m); dens.append(den); nums.append(num)
    ms = jnp.stack(ms); dens = jnp.stack(dens); nums = jnp.stack(nums)
    w = jnp.exp(ms - jnp.max(ms, axis=0, keepdims=True))
    out = jnp.sum(w[..., None] * nums, axis=0) / jnp.sum(w * dens, axis=0)[..., None]
    return out.transpose(0, 2, 1, 3).reshape(B, S, A_WIDTH).astype(qa.dtype)


def hgrn2(qb, fb, ib, gb, lb, g_norm):
    B, S, _ = qb.shape
    heads = lambda t: t.astype(jnp.float32).reshape(B, S, B_HEADS, -1)
    lb = lb.astype(jnp.float32).reshape(B_HEADS, B_EXPAND)
    q = heads(qb)
    f = lb + (1.0 - lb) * jax.nn.sigmoid(heads(fb))
    k = 1.0 - f
    logf = jnp.log(f)
    v = heads(ib)
    nc = S // B_CHUNK
    chunks = lambda t: t.reshape(B, nc, B_CHUNK, B_HEADS, -1).transpose(1, 0, 3, 2, 4)
    tril = jnp.tril(jnp.ones((B_CHUNK, B_CHUNK), dtype=bool))[:, :, None]

    def step(state, inp):
        qc, kc, vc, gc = inp
        b = jnp.cumsum(gc, axis=2)
        inter = jnp.einsum("bhtk,bhkv->bhtv", qc * jnp.exp(b), state)
        diff = b[:, :, :, None, :] - b[:, :, None, :, :]
        decay = jnp.exp(jnp.where(tril, diff, -jnp.inf))
        scores = jnp.einsum("bhtk,bhsk,bhtsk->bhts", qc, kc, decay)
        intra = jnp.einsum("bhts,bhsv->bhtv", scores, vc)
        b_end = b[:, :, -1, :]
        new_state = jnp.exp(b_end)[..., None] * state + jnp.einsum(
            "bhsk,bhsv->bhkv", kc * jnp.exp(b_end[:, :, None, :] - b), vc)
        return new_state, inter + intra

    state0 = jnp.zeros((B, B_HEADS, B_EXPAND, B_HEAD_V), jnp.float32)
    _, o = lax.scan(step, state0, (chunks(q), chunks(k), chunks(v), chunks(logf)))
    o = o.transpose(1, 0, 3, 2, 4).reshape(B, S, B_HEADS, B_HEAD_V)
    o = rmsnorm(o, g_norm) * jax.nn.silu(heads(gb))
    return o.reshape(B, S, B_WIDTH).astype(qb.dtype)


def setup_inputs(seed: int = 0) -> dict:
    key = jax.random.key(seed)
    ks = jax.random.split(key, 20)
    nrm = lambda k, shape, fan_in: jax.random.normal(k, shape, jnp.float32) * (fan_in ** -0.5)
    gain = lambda k, shape: 1.0 + 0.01 * jax.random.normal(k, shape, jnp.float32)
    return {
        "x": jax.random.normal(ks[0], (BATCH, SEQ, D_MODEL), jnp.float32),
        "p": jax.random.normal(ks[1], (DEPTH, BATCH, SEQ, PLE_DIM), jnp.float32),
        "norm_mix": gain(ks[2], (DEPTH, D_MODEL)),
        "w_in": nrm(ks[3], (DEPTH, D_MODEL, IN_TOTAL), D_MODEL),
        "hg_lb": 0.1 * jax.random.normal(ks[4], (DEPTH + 1, B_WIDTH_K), jnp.float32),
        "hg_norm": gain(ks[5], (DEPTH, B_HEAD_V)),
        "w_a_up": nrm(ks[6], (DEPTH, A_WIDTH, D_MODEL), A_WIDTH),
        "w_b_up": nrm(ks[7], (DEPTH, B_WIDTH, D_MODEL), B_WIDTH),
        "w_out": nrm(ks[8], (DEPTH, D_MODEL, D_MODEL), D_MODEL),
        "norm_ffn": gain(ks[9], (DEPTH, D_MODEL)),
        "w_gu": nrm(ks[10], (DEPTH, D_MODEL, 2 * FFN_HIDDEN), D_MODEL),
        "w_down": nrm(ks[11], (DEPTH, FFN_HIDDEN, D_MODEL), FFN_HIDDEN),
        "norm_ple": gain(ks[12], (DEPTH, D_MODEL)),
        "w_pe": nrm(ks[13], (DEPTH, PLE_DIM, D_MODEL), PLE_DIM),
        "w_pg": nrm(ks[14], (DEPTH, D_MODEL, D_MODEL), D_MODEL),
        "norm_final": gain(ks[15], (D_MODEL,)),
    }


def reference(x, p, norm_mix, w_in, hg_lb, hg_norm, w_a_up, w_b_up, w_out, norm_ffn,
              w_gu, w_down, norm_ple, w_pe, w_pg, norm_final):
    lb_all = jnp.cumsum(jax.nn.softmax(hg_lb.astype(jnp.float32), axis=0), axis=0)
    for i in range(DEPTH):
        h = rmsnorm(x, norm_mix[i])
        proj = h @ w_in[i]
        qa, ka, va, qb, fb, ib, gb, gate_a, gate_b = jnp.split(proj, IN_SPLITS, axis=-1)
        ya = dilated_attention(qa, ka, va)
        yb = hgrn2(qb, fb, ib, gb, lb_all[i], hg_norm[i])
        merged = jax.nn.sigmoid(gate_a) * (ya @ w_a_up[i]) + jax.nn.sigmoid(gate_b) * (yb @ w_b_up[i])
        x = x + merged @ w_out[i]
        h = rmsnorm(x, norm_ffn[i])
        g, u = jnp.split(h @ w_gu[i], 2, axis=-1)
        x = x + (jax.nn.silu(g) * u) @ w_down[i]
        hp = rmsnorm(x, norm_ple[i])
        x = x + (p[i] @ w_pe[i]) * jax.nn.sigmoid(hp @ w_pg[i])
    return rmsnorm(x, norm_final)
```

```python
from contextlib import ExitStack

import numpy as np
import concourse.bass as bass
import concourse.mybir as mybir
from concourse.bass_utils import run_bass_kernel_spmd

F32 = mybir.dt.float32
BF16 = mybir.dt.bfloat16
AF = mybir.ActivationFunctionType
ALU = mybir.AluOpType

NCORES = 8
SEQ = 4096
D = 1024
T = 512
NT = SEQ // T
NSEQ = 2
FFN = 2816
EPS = 1e-6
VW = 520
RING = 4
USZ = 4096

C_ID = 0
C_G = 128
C_HGN = 160
C_LB = 161
C_EPS = 169
NCF = 170
M_CUR = 0
M_PREV = 512
M_HG = 1024
M_C16 = 1536
M_P16 = 3584
M_ONE = 5632
M_SEL = 5760
M_SEG = 5888
NMK = 6400

U_IN = 0
U_MG = 11
U_OUT = 19
U_GU = 21
U_DN = 32
U_PG = 40
U_PE = 42
NU = 43


class _RecInst:
    def then_inc(self, *a, **k):
        return self


class _Rec:
    def __init__(self):
        self.calls = []

    def __getattr__(self, name):
        def f(*a, **kw):
            self.calls.append((name, a, kw))
            return _RecInst()
        return f


def _fsz(ap):
    n = 1
    for d in tuple(ap.shape)[1:]:
        n *= int(d)
    return n


def _est(eng, calls):
    t = 0.0
    lat = 0.0
    for name, a, kw in calls:
        if name == "matmul":
            rhs = kw.get("rhs")
            n = _fsz(rhs)
            mult = 4.0 if rhs.dtype == F32 else 1.0
            if n >= 512:
                t += n * mult / 1.95 + 16.0
            else:
                t += max(64, n) * mult / 1.95 + 46.0
        elif name == "transpose":
            t += 128 * 2.0 / 1.95 + 16.0
        elif name == "dma_start":
            out = kw.get("out")
            nbytes = 4 * int(out.shape[0]) * _fsz(out)
            t += 1500.0 if eng == "pool" else 150.0
            lat = max(lat, 2500.0 + nbytes / 120.0)
        elif name == "activation":
            t += max(64, _fsz(kw.get("in_"))) * 0.83 + 220.0
        elif name == "tensor_tensor_scan":
            t += 2 * _fsz(kw.get("data0")) * 1.04 + 200.0
        else:
            ap = kw.get("out")
            if ap is None:
                ap = a[0]
            n = _fsz(ap)
            if eng == "pool":
                t += n * 1.7 + 350.0
            else:
                t += max(64, n) * 1.04 + 200.0
    return t, lat


class Sched:
    SYNC = 250.0

    def schedule(self, engs):
        ops = self.ops
        n = len(ops)
        cost = [0.0] * n
        lat = [0.0] * n
        for i, o in enumerate(ops):
            rec = _Rec()
            o["fn"](rec)
            cost[i], lat[i] = _est(o["eng"], rec.calls)
        dependents = [[] for _ in range(n)]
        ndeps = [0] * n
        for i, o in enumerate(ops):
            for j in o["deps"]:
                dependents[j].append(i)
            ndeps[i] = len(o["deps"])
        ready = [0.0] * n
        avail = {e: [] for e in engs}
        for i in range(n):
            if ndeps[i] == 0:
                avail[ops[i]["eng"]].append(i)
        free = {e: 0.0 for e in engs}
        order = {e: [] for e in engs}
        busy = {e: 0.0 for e in engs}
        done = 0
        tend = 0.0
        while done < n:
            best = None
            for e in engs:
                av = avail[e]
                if not av:
                    continue
                t = free[e]
                bk_ = None
                bi = -1
                for i in av:
                    r = ready[i]
                    k_ = (r if r > t else t, i)
                    if bk_ is None or k_ < bk_:
                        bk_ = k_
                        bi = i
                if best is None or bk_ < best[0]:
                    best = (bk_, bi, e)
            (st, _), i, e = best
            avail[e].remove(i)
            ops[i]["st"] = st
            ops[i]["cost"] = cost[i]
            fin = st + cost[i]
            free[e] = fin
            busy[e] += cost[i]
            order[e].append(i)
            fin += lat[i]
            ops[i]["fin"] = fin
            tend = max(tend, fin)
            for d in dependents[i]:
                if fin + self.SYNC > ready[d]:
                    ready[d] = fin + self.SYNC
                ndeps[d] -= 1
                if ndeps[d] == 0:
                    avail[ops[d]["eng"]].append(d)
            done += 1
        self.model_ns = tend
        self.model_busy = busy
        return order

    def __init__(self):
        self.ops = []
        self.last_w = {}
        self.readers = {}

    def add(self, eng, fn, reads=(), writes=(), dkey=None, ndma=1):
        i = len(self.ops)
        deps = set()
        writes = list(writes) + [k for k in reads if isinstance(k, tuple) and k[0] == "ps"]
        for k in reads:
            if k in self.last_w:
                deps.add(self.last_w[k])
        for k in writes:
            if k in self.last_w:
                deps.add(self.last_w[k])
            deps.update(self.readers.get(k, ()))
        for k in reads:
            self.readers.setdefault(k, []).append(i)
        for k in writes:
            self.last_w[k] = i
            self.readers[k] = []
        deps.discard(i)
        self.ops.append(dict(eng=eng, fn=fn, deps=deps, dkey=dkey, ndma=ndma, tag=getattr(self, "tag", "")))
        return i

    def emit(self, nc, stack, final_engine="sp", reorder=True):
        ops = self.ops
        engs = ["pe", "act", "dve", "pool", "sp"]
        if reorder:
            per_eng = self.schedule(engs)
        else:
            per_eng = {e: [] for e in engs}
            for i, o in enumerate(ops):
                per_eng[o["eng"]].append(i)
        needed = set()
        for i, o in enumerate(ops):
            for j in o["deps"]:
                oj = ops[j]
                if oj["dkey"] is None and oj["eng"] == "pe" and o["eng"] == "pe" and o["dkey"] is None:
                    continue
                needed.add(j)
        esem = {e: stack.enter_context(nc.semaphore("s_" + e)) for e in engs}
        dsem = {}
        dtot = {}
        cnt = {e: 0 for e in engs}
        sig = {}
        out_waits = []
        for i, o in enumerate(ops):
            if o["dkey"] is not None:
                k = o["dkey"]
                if k not in dsem:
                    dsem[k] = stack.enter_context(nc.semaphore("d_%d" % len(dsem)))
                    dtot[k] = 0
                dtot[k] += 16 * o["ndma"]
                sig[i] = (dsem[k], dtot[k])
        for e in engs:
            for i in per_eng[e]:
                if ops[i]["dkey"] is None and i in needed:
                    cnt[e] += 1
                    sig[i] = (esem[e], cnt[e])
        self.nsem = 5 + len(dsem)
        self.counts = dict(cnt)
        final = {}
        for k in dsem:
            if isinstance(k, tuple) and k[0] == "out":
                final[k] = (dsem[k], dtot[k])

        def run_engine(ename, eobj):
            waited = {}
            for i in per_eng[ename]:
                o = ops[i]
                w = {}
                for j in o["deps"]:
                    if j not in sig:
                        continue
                    oj = ops[j]
                    if oj["dkey"] is None and oj["eng"] == "pe" and ename == "pe" and o["dkey"] is None:
                        continue
                    s, v = sig[j]
                    if w.get(s.num, (None, 0))[1] < v:
                        w[s.num] = (s, v)
                for num, (s, v) in w.items():
                    if waited.get(num, 0) >= v:
                        continue
                    eobj.wait_ge(s, v)
                    waited[num] = v
                r = o["fn"](eobj)
                if o["dkey"] is not None:
                    insts = r if isinstance(r, (list, tuple)) else [r]
                    assert len(insts) == o["ndma"], (len(insts), o["ndma"])
                    for ins in insts:
                        ins.then_inc(sig[i][0], 16)
                elif i in sig:
                    r.then_inc(sig[i][0], 1)
            if ename == final_engine:
                for k, (s, v) in final.items():
                    eobj.wait_ge(s, v)

        block = stack.enter_context(nc.Block())

        @block.tensor
        def _(e):
            run_engine("pe", e)

        @block.scalar
        def _(e):
            run_engine("act", e)

        @block.vector
        def _(e):
            run_engine("dve", e)

        @block.gpsimd
        def _(e):
            run_engine("pool", e)

        @block.sync
        def _(e):
            run_engine("sp", e)


class _Stop(Exception):
    pass


def build_program(nseq=NSEQ, ntiles=NT, stop=None):
    nc = bass.Bass("TRN2", target_bir_lowering=False)
    x_d = nc.dram_tensor("x", [NSEQ * SEQ, D], F32, kind="ExternalInput").ap()
    p_d = nc.dram_tensor("p", [NSEQ * SEQ, 256], F32, kind="ExternalInput").ap()
    wp_d = nc.dram_tensor("wpack", [NU, 128, USZ], F32, kind="ExternalInput").ap()
    cst_d = nc.dram_tensor("cst", [128, NCF], F32, kind="ExternalInput").ap()
    cmk_d = nc.dram_tensor("cmask", [128, NMK], F32, kind="ExternalInput").ap()
    out_d = nc.dram_tensor("out", [NSEQ * SEQ, D], F32, kind="ExternalOutput").ap()
    vscr = nc.dram_tensor("vscr", [NSEQ, SEQ, VW], BF16, kind="Internal").ap()

    S = Sched()
    stack = ExitStack()

    def sb(name, shape, dt):
        return stack.enter_context(nc.sbuf_tensor("sb_" + name, shape, dt))

    cst = sb("cst", [128, NCF], F32)
    cmk = sb("cmk", [128, NMK], BF16)
    lbc = sb("lbc", [128, 8], F32)
    kT = sb("kT", [128, 4, SEQ], BF16)
    v16 = sb("v16", [128, 2, 16, VW], BF16)
    v4 = sb("v4", [128, 2, 4, VW], BF16)
    v1 = sb("v1", [128, 2, 4, VW], BF16)
    wr = sb("wr", [128, RING, USZ], BF16)
    xT = sb("xT", [128, 8, T], F32)
    pT = sb("pT", [128, 2, T], BF16)
    hT = sb("hT", [128, 8, T], BF16)
    qTe = sb("qTe", [128, 4, T], BF16)
    qTo = sb("qTo", [128, 4, T], BF16)
    ktmA = sb("ktmA", [128, 4, 128], BF16)
    ktmB = sb("ktmB", [128, 4, 128], BF16)
    hib = sb("hib", [128, T], BF16)
    lob = sb("lob", [128, T], BF16)
    yaT = sb("yaT", [128, 4, T], BF16)
    ybT = sb("ybT", [128, 4, T], BF16)
    vtb = sb("vtb", [128, 4, 512], BF16)
    St = sb("St", [128, 4, 128], F32)
    Sbf = sb("Sbf", [128, 8, 128], BF16)
    ebe = sb("ebe", [128, 16], F32)
    sq = sb("sq", [128, 2, T], BF16)
    R = sb("R", [128, 12288], BF16)
    ps = stack.enter_context(nc.psum_tensor("psum_all", [128, 8, 512], F32))

    def Rk(s0, n):
        return [("R", s) for s in range(s0, s0 + n)]

    def Rbf(s0, n):
        return R[:, s0 * 512:(s0 + n) * 512]

    def Rf(s0, n):
        return R[:, s0 * 512:(s0 + n) * 512].bitcast(F32)

    rstd = Rf(22, 2)
    rstdk = Rk(22, 2)

    free_banks = list(range(8))

    bpool = [None]
    pt_ctr = [0]

    def balloc():
        allowed = bpool[0]
        for idx, b in enumerate(free_banks):
            if allowed is None or b in allowed:
                return free_banks.pop(idx)
        raise AssertionError("out of PSUM banks")

    def bfree(b):
        free_banks.append(b)

    def bank(b):
        return ps[:, b, :]

    def bk(b):
        return [("ps", b)]

    ident = cst[:, C_ID:C_ID + 128]
    epsc = cst[:, C_EPS:C_EPS + 1]
    segm = cmk[:, M_SEG:M_SEG + 512]
    onesb = cmk[:, M_ONE:M_ONE + 128]

    S.add("sp", lambda e: e.dma_start(out=cst[:], in_=cst_d), writes=["cst"], dkey=("c", 0))
    S.add("pool", lambda e: e.dma_start(out=cmk[:], in_=cmk_d), writes=["cmk"], dkey=("c", 1))
    S.add("dve", lambda e: e.tensor_tensor(out=lbc[:, 0:4], in0=cst[:, C_LB:C_LB + 4], in1=cst[:, C_LB + 4:C_LB + 8],
                                           op=ALU.subtract), reads=["cst"], writes=["lbc"])
    S.add("act", lambda e: e.activation(out=lbc[:, 0:4], in_=lbc[:, 0:4], func=AF.Sigmoid), reads=["lbc"], writes=["lbc"])
    S.add("dve", lambda e: e.tensor_scalar(out=lbc[:, 4:8], in0=lbc[:, 0:4], scalar1=-1.0, scalar2=1.0,
                                           op0=ALU.mult, op1=ALU.add), reads=["lbc"], writes=["lbc2"])
    v1v = v1[:].rearrange("p a b (h c) -> p (a b h) c", c=65)
    S.add("pool", lambda e: e.memset(v1v[:, :, 64:65], 1.0), writes=[("v1", 0), ("v1", 1)])
    S.add("pool", lambda e: e.memset(qTe[64:128, :, :], 0.0), writes=["qTe_z"])
    S.add("pool", lambda e: e.memset(qTo[0:64, :, :], 0.0), writes=["qTo_z"])
    S.add("pool", lambda e: e.memset(ktmA[64:128, :, :], 0.0), writes=["ktmA_z"])
    S.add("pool", lambda e: e.memset(ktmB[0:64, :, :], 0.0), writes=["ktmB_z"])
    S.add("dve", lambda e: e.memset(hib[:], 0.0), writes=["hib"])
    S.add("dve", lambda e: e.memset(lob[:], 0.0), writes=["lob"])
    for hp_ in range(4):
        S.add("pool", lambda e, hp_=hp_: e.memset(kT[:, hp_, :], 0.0), writes=[("kT", hp_, tt) for tt in range(NT)])
    for pp_ in range(2):
        S.add("dve", lambda e, pp_=pp_: e.memset(v16[:, pp_, :, :], 0.0), writes=[("v16", pp_, jj) for jj in range(4)])

    unit_seq = []
    per_tile_units = (
        [(U_IN + u, 4096) for u in (0, 1, 2, 5, 3, 4, 6)]
        + [(U_MG + c, 3072) for c in range(8)]
        + [(U_OUT + u, 4096) for u in range(2)]
        + [(U_GU + u, 4096) for u in range(11)]
        + [(U_DN + c, 2816) for c in range(8)]
        + [(U_PG + u, 4096) for u in range(2)]
        + [(U_PE, 2048)]
    )
    for _ in range(nseq * ntiles):
        unit_seq.extend(per_tile_units)
    wstate = dict(issued=0, consumed=0, released=0)

    def w_issue():
        i = wstate["issued"]
        if i >= len(unit_seq):
            return
        uid, n = unit_seq[i]
        slot = i % RING
        S.add("pool", (lambda e, slot=slot, uid=uid, n=n: e.dma_start(out=wr[:, slot, 0:n], in_=wp_d[uid, :, 0:n])),
              writes=[("w", slot)], dkey=("w", slot))
        wstate["issued"] += 1

    def w_next(expect_uid):
        i = wstate["consumed"]
        uid, n = unit_seq[i]
        assert uid == expect_uid, (uid, expect_uid)
        assert wstate["issued"] > i and i - wstate["released"] < RING
        wstate["consumed"] += 1
        slot = i % RING
        return wr[:, slot, :], [("w", slot)]

    def w_release(k=1):
        for _ in range(k):
            assert wstate["released"] < wstate["consumed"]
            wstate["released"] += 1
            w_issue()

    for _ in range(RING):
        w_issue()

    def rmsnorm(gidx, out_fn, out_keys_fn):
        b = balloc()
        for c in range(8):
            sl = c % 2
            S.add("act", lambda e, c=c, sl=sl: e.activation(out=sq[:, sl, :], in_=xT[:, c, :], func=AF.Square),
                  reads=[("xT", c)], writes=[("sq", sl)])
            S.add("pe", lambda e, c=c, sl=sl: e.matmul(bank(b), lhsT=onesb, rhs=sq[:, sl, :], start=(c == 0), stop=(c == 7)),
                  reads=[("sq", sl), "cmk"], writes=bk(b))
        S.add("act", lambda e: e.activation(out=rstd[:], in_=bank(b), func=AF.Ln, bias=epsc, scale=1.0 / D),
              reads=bk(b) + ["cst"], writes=rstdk)
        bfree(b)
        S.add("act", lambda e: e.activation(out=rstd[:], in_=rstd[:], func=AF.Exp, scale=-0.5),
              reads=rstdk, writes=rstdk)
        for c in range(8):
            S.add("dve", lambda e, c=c: e.scalar_tensor_tensor(out=out_fn(c), in0=xT[:, c, :],
                                                              scalar=cst[:, C_G + gidx * 8 + c:C_G + gidx * 8 + c + 1],
                                                              in1=rstd[:], op0=ALU.mult, op1=ALU.mult),
                  reads=[("xT", c), "cst"] + rstdk, writes=out_keys_fn(c))

    def dense_T(b, lhs_fn, nk, rhs_fn, rkeys_fn, wkeys, N=T, split=False):
        if split:
            for k in range(nk):
                S.add("pe", (lambda e, k=k: e.matmul(bank(b)[:, 0:N], lhsT=lhs_fn(k), rhs=rhs_fn(k), start=(k == 0),
                                                     stop=(k == nk - 1))), reads=rkeys_fn(k) + wkeys, writes=bk(b))
            return

        def fn(e):
            r = None
            for k in range(nk):
                r = e.matmul(bank(b)[:, 0:N], lhsT=lhs_fn(k), rhs=rhs_fn(k), start=(k == 0), stop=(k == nk - 1))
            return r
        rk = []
        for k in range(nk):
            rk += rkeys_fn(k)
        S.add("pe", fn, reads=rk + wkeys, writes=bk(b))

    hkeys = lambda k: [("hT", k)]

    try:
      for s in range(nseq):
          for ti in range(ntiles):
              t0 = ti * T
              row0 = s * SEQ + t0
              par = ti % 2
              n16 = ti // 4
              j16 = ti % 4

              S.tag = 't%d.%d:load' % (s, ti)
              bpool[0] = (0, 1, 2, 3)
              xin = Rf(0, 16).rearrange("p (b d) -> p b d", d=D)
              pin = Rf(16, 4).rearrange("p (b d) -> p b d", d=256)
              for blk in range(4):
                  S.add("sp", lambda e, xin=xin, row0=row0, blk=blk: e.dma_start(
                      out=xin[:, blk, :], in_=x_d[row0 + 128 * blk:row0 + 128 * (blk + 1), :]),
                      writes=Rk(4 * blk, 4), dkey=("xin", blk))
              S.add("sp", lambda e, pin=pin, row0=row0: e.dma_start(
                  out=pin, in_=p_d[row0:row0 + T, :].rearrange("(b p) d -> p b d", p=128)),
                  writes=Rk(16, 4), dkey=("pin", 0))
              for blk in range(4):
                  for hh in range(2):
                      b = balloc()

                      def fn(e, hh=hh, b=b, xin=xin, blk=blk):
                          r = None
                          for cc in range(4):
                              c = 4 * hh + cc
                              r = e.transpose(bank(b)[:, cc * 128:(cc + 1) * 128], xin[:, blk, c * 128:(c + 1) * 128], ident)
                          return r
                      S.add("pe", fn, reads=Rk(4 * blk, 4) + ["cst"], writes=bk(b))
                      dst = xT[:, 4 * hh:4 * hh + 4, blk * 128:(blk + 1) * 128]
                      src = bank(b).rearrange("p (c t) -> p c t", t=128)
                      if hh == 0:
                          S.add("act", lambda e, dst=dst, src=src: e.activation(out=dst, in_=src, func=AF.Copy),
                                reads=bk(b), writes=[("xT", 4 * hh + cc) for cc in range(4)])
                      else:
                          S.add("dve", lambda e, dst=dst, src=src: e.tensor_copy(out=dst, in_=src),
                                reads=bk(b), writes=[("xT", 4 * hh + cc) for cc in range(4)])
                      bfree(b)
              for c in range(2):
                  b = balloc()

                  def fn(e, c=c, b=b, pin=pin):
                      r = None
                      for blk in range(4):
                          r = e.transpose(bank(b)[:, blk * 128:(blk + 1) * 128], pin[:, blk, c * 128:(c + 1) * 128], ident)
                      return r
                  S.add("pe", fn, reads=Rk(16, 4) + ["cst"], writes=bk(b))
                  S.add("dve", lambda e, c=c, b=b: e.tensor_copy(out=pT[:, c, :], in_=bank(b)),
                        reads=bk(b), writes=[("pT", c)])
                  bfree(b)

              S.tag = 't%d.%d:proj' % (s, ti)
              rmsnorm(0, lambda c: hT[:, c, :], lambda c: [("hT", c)])

              wq, wqk = w_next(U_IN + 0)
              wqv = wq.rearrange("p (k c) -> p k c", c=512)
              for hp in range(4):
                  b = balloc()
                  dense_T(b, lambda k, hp=hp, wqv=wqv: wqv[:, k, hp * 128:(hp + 1) * 128], 8, lambda k: hT[:, k, :], hkeys, wqk, split=(hp == 0))
                  S.add("act", lambda e, hp=hp, b=b: e.activation(out=qTe[0:64, hp, :], in_=bank(b)[0:64, :], func=AF.Copy),
                        reads=bk(b) + ["qTe_z"], writes=[("qTe", hp)])
                  S.add("dve", lambda e, hp=hp, b=b: e.tensor_copy(out=qTo[64:128, hp, :], in_=bank(b)[64:128, :]),
                        reads=bk(b) + ["qTo_z"], writes=[("qTo", hp)])
                  bfree(b)
              w_release()
              wk_, wkk = w_next(U_IN + 1)
              wkv = wk_.rearrange("p (k c) -> p k c", c=512)
              for hp in range(4):
                  b = balloc()
                  dense_T(b, lambda k, hp=hp, wkv=wkv: wkv[:, k, hp * 128:(hp + 1) * 128], 8, lambda k: hT[:, k, :], hkeys, wkk)
                  S.add("dve", lambda e, hp=hp, b=b, t0=t0: e.tensor_copy(out=kT[:, hp, t0:t0 + T], in_=bank(b)),
                        reads=bk(b), writes=[("kT", hp, ti)])
                  bfree(b)
              w_release()
              wv_, wvk = w_next(U_IN + 2)
              wvv = wv_.rearrange("p (k c) -> p k c", c=512)
              for blk in range(4):
                  b = balloc()
                  dense_T(b, lambda k, blk=blk: hT[:, k, blk * 128:(blk + 1) * 128], 8, lambda k, wvv=wvv: wvv[:, k, :],
                          lambda k: [], wvk + [("hT", k) for k in range(8)])
                  dst = v1[:, par, blk, :].rearrange("p (h c) -> p h c", c=65)[:, :, 0:64]
                  S.add("act", lambda e, b=b, dst=dst: e.activation(out=dst, in_=bank(b).rearrange("p (h c) -> p h c", c=64), func=AF.Copy),
                        reads=bk(b), writes=[("v1", par)])
                  bfree(b)
              w_release()
              S.add("sp", lambda e, s=s, t0=t0, par=par: e.dma_start(
                  out=vscr[s, t0:t0 + T, :].rearrange("(b p) c -> p b c", p=128), in_=v1[:, par, :, :]),
                  reads=[("v1", par)], writes=[("vscr", s, ti), "vsq"], dkey=("vs", 0))
              S.add("sp", lambda e, s=s, t0=t0, par=par: e.dma_start(
                  out=v4[:, par, :, :], in_=vscr[s, t0:t0 + T, :].rearrange("(a r) c -> a r c", r=4)),
                  reads=[("vscr", s, ti)], writes=[("v4", par)], dkey=("v4", par))
              S.add("sp", lambda e, s=s, t0=t0, n16=n16, j16=j16: e.dma_start(
                  out=v16[32 * j16:32 * j16 + 32, n16 % 2, :, :], in_=vscr[s, t0:t0 + T, :].rearrange("(a r) c -> a r c", r=16)),
                  reads=[("vscr", s, ti)], writes=[("v16", n16 % 2, j16)], dkey=("v16", n16 % 2, j16))
              wi_, wik = w_next(U_IN + 5)
              wiv = wi_.rearrange("p (k c) -> p k c", c=512)
              for blk in range(4):
                  b = balloc()
                  dense_T(b, lambda k, blk=blk: hT[:, k, blk * 128:(blk + 1) * 128], 8, lambda k, wiv=wiv: wiv[:, k, :],
                          lambda k: [], wik + [("hT", k) for k in range(8)])
                  S.add("dve", lambda e, b=b, blk=blk: e.tensor_copy(out=vtb[:, blk, :], in_=bank(b)),
                        reads=bk(b), writes=[("vtb", blk)])
                  bfree(b)

              w_release()
              if stop == 'A':
                  raise _Stop()
              S.tag = 't%d.%d:hgrn' % (s, ti)
              wqb_, wqbk = w_next(U_IN + 3)
              wfb_, wfbk = w_next(U_IN + 4)
              wgb_, wgbk = w_next(U_IN + 6)
              wqbv = wqb_.rearrange("p (k c) -> p k c", c=512)
              wfbv = wfb_.rearrange("p (k c) -> p k c", c=512)
              wgbv = wgb_.rearrange("p (k c) -> p k c", c=512)
              ktmk = ["ktmA", "ktmB"]
              Am, Amk = Rbf(9, 1), Rk(9, 1)
              osq, osqk = Rbf(10, 1), Rk(10, 1)

              def hgrn_head(h, T1, T1k, T2, T2k, T3, T3k, qe, qek, ke, kek, ebe_, ebek):
                  bA, bB, bC = balloc(), balloc(), balloc()
                  dense_T(bA, lambda k, h=h, wqbv=wqbv: wqbv[:, k, h * 128:(h + 1) * 128], 8, lambda k: hT[:, k, :], hkeys, wqbk)
                  dense_T(bB, lambda k, h=h, wfbv=wfbv: wfbv[:, k, h * 128:(h + 1) * 128], 8, lambda k: hT[:, k, :], hkeys, wfbk)
                  dense_T(bC, lambda k, h=h, wgbv=wgbv: wgbv[:, k, h * 128:(h + 1) * 128], 8, lambda k: hT[:, k, :], hkeys, wgbk)
                  S.add("act", lambda e, bC=bC: e.activation(out=T2, in_=bank(bC), func=AF.Sigmoid), reads=bk(bC), writes=T2k)
                  S.add("dve", lambda e, bC=bC, h=h: e.tensor_tensor(out=ybT[:, h, :], in0=bank(bC), in1=T2, op=ALU.mult),
                        reads=bk(bC) + T2k, writes=[("ybT", h)])
                  bfree(bC)
                  S.add("act", lambda e, bB=bB: e.activation(out=T1, in_=bank(bB), func=AF.Sigmoid), reads=bk(bB), writes=T1k)
                  bfree(bB)
                  S.add("dve", lambda e, h=h: e.tensor_scalar(out=T1, in0=T1, scalar1=lbc[:, 4 + h:5 + h], scalar2=lbc[:, h:h + 1],
                                                             op0=ALU.mult, op1=ALU.add), reads=T1k + ["lbc", "lbc2"], writes=T1k)
                  S.add("act", lambda e: e.activation(out=T2, in_=T1, func=AF.Ln), reads=T1k, writes=T2k)
                  S.add("dve", lambda e: e.tensor_tensor_scan(out=T3, data0=segm, data1=T2, initial=0.0, op0=ALU.mult, op1=ALU.add),
                        reads=T2k + ["cmk"], writes=T3k)
                  S.add("dve", lambda e: e.tensor_scalar(out=T1, in0=T1, scalar1=-1.0, scalar2=1.0, op0=ALU.mult, op1=ALU.add),
                        reads=T1k, writes=T1k)
                  S.add("act", lambda e: e.activation(out=T2, in_=T3, func=AF.Exp), reads=T3k, writes=T2k)
                  S.add("dve", lambda e, bA=bA: e.tensor_tensor(out=qe, in0=bank(bA), in1=T2, op=ALU.mult), reads=bk(bA) + T2k, writes=qek)
                  bfree(bA)
                  S.add("dve", lambda e: e.tensor_copy(out=ebe_, in_=T2.rearrange("p (c k) -> p c k", k=64)[:, :, 63]),
                        reads=T2k, writes=[ebek])
                  S.add("act", lambda e: e.activation(out=T2, in_=T3, func=AF.Exp, scale=-1.0), reads=T3k + [ebek], writes=T2k)
                  S.add("dve", lambda e: e.tensor_tensor(out=ke, in0=T1, in1=T2, op=ALU.mult), reads=T1k + T2k, writes=kek)
                  T3v = T3.rearrange("p (c k) -> p c k", k=64)
                  T2v = T2.rearrange("p (c k) -> p c k", k=64)
                  S.add("dve", lambda e, T3v=T3v, T2v=T2v: e.tensor_tensor(out=T2v, in0=T3v[:, :, 63:64].broadcast_to([128, 8, 64]),
                                                                           in1=T3v, op=ALU.subtract), reads=T3k, writes=T2k)
                  S.add("act", lambda e: e.activation(out=T2, in_=T2, func=AF.Exp), reads=T2k, writes=T2k)
                  S.add("dve", lambda e: e.tensor_tensor(out=T2, in0=T1, in1=T2, op=ALU.mult), reads=T1k + T2k, writes=T2k)
                  if stop == 'B1':
                      raise _Stop()
                  bD = balloc()

                  def fnT(e, bD=bD):
                      r = None
                      for blk in range(4):
                          r = e.transpose(bank(bD)[:, blk * 128:(blk + 1) * 128], T2[:, blk * 128:(blk + 1) * 128], ident)
                      return r
                  S.add("pe", fnT, reads=T2k + ["cst"], writes=bk(bD))
                  S.add("act", lambda e, bD=bD: e.activation(out=ktmA[0:64, :, :], in_=bank(bD)[0:64, :].rearrange("p (b k) -> p b k", k=128),
                                                             func=AF.Copy), reads=bk(bD) + ["ktmA_z"], writes=["ktmA"])
                  S.add("dve", lambda e, bD=bD: e.tensor_copy(out=ktmB[64:128, :, :], in_=bank(bD)[64:128, :].rearrange("p (b k) -> p b k", k=128)),
                        reads=bk(bD) + ["ktmB_z"], writes=["ktmB"])
                  bfree(bD)
                  if stop == 'B2':
                      raise _Stop()
                  bE = balloc()

                  def fnA(e, bE=bE):
                      r = None
                      for blk in range(4):
                          sl = slice(blk * 128, (blk + 1) * 128)
                          r = e.matmul(bank(bE)[:, sl], lhsT=ke[:, sl], rhs=qe[:, sl], start=True, stop=True)
                      return r
                  S.add("pe", fnA, reads=kek + qek, writes=bk(bE))
                  S.add("dve", lambda e, bE=bE: e.tensor_tensor(out=Am, in0=bank(bE), in1=cmk[:, M_HG:M_HG + 512], op=ALU.mult),
                        reads=bk(bE) + ["cmk"], writes=Amk)
                  bfree(bE)
                  if stop == 'B3':
                      raise _Stop()
                  bF, bG = balloc(), balloc()

                  def fnU(e, bF=bF, bG=bG, h=h):
                      r = None
                      for c in range(8):
                          blk, half = c // 2, c % 2
                          bb = bF if half == 0 else bG
                          r = e.matmul(bank(bb)[:, blk * 128:(blk + 1) * 128],
                                       lhsT=(ktmA if half == 0 else ktmB)[:, blk, :],
                                       rhs=vtb[:, blk, h * 128:(h + 1) * 128], start=True, stop=True)
                      return r
                  S.add("pe", fnU, reads=ktmk + [("vtb", blk) for blk in range(4)], writes=bk(bF) + bk(bG))
                  if ti == 0:
                      S.add("dve", lambda e, h=h: e.memset(St[:, h, :], 0.0), writes=[("St", h)])
                  for c in range(8):
                      S.add("act", lambda e, c=c, h=h: e.activation(out=Sbf[:, c, :], in_=St[:, h, :], func=AF.Copy),
                            reads=[("St", h)], writes=[("Sbf", c)])
                      bb = bF if c % 2 == 0 else bG
                      S.add("dve", lambda e, c=c, h=h, bb=bb: e.scalar_tensor_tensor(
                          out=St[:, h, :], in0=St[:, h, :], scalar=ebe_[:, c:c + 1],
                          in1=bank(bb)[:, (c // 2) * 128:(c // 2 + 1) * 128], op0=ALU.mult, op1=ALU.add),
                          reads=[("St", h), ebek] + bk(bb), writes=[("St", h)])
                  bfree(bF)
                  bfree(bG)
                  if stop == 'B4':
                      raise _Stop()
                  bH = balloc()

                  def fnO(e, bH=bH, h=h):
                      r = None
                      for blk in range(4):
                          sl = slice(blk * 128, (blk + 1) * 128)
                          e.matmul(bank(bH)[:, sl], lhsT=vtb[:, blk, h * 128:(h + 1) * 128], rhs=Am[:, sl], start=True, stop=False)
                          for half in range(2):
                              c = 2 * blk + half
                              cs = slice(c * 64, (c + 1) * 64)
                              r = e.matmul(bank(bH)[:, cs], lhsT=Sbf[:, c, :], rhs=qe[:, cs], start=False, stop=(half == 1))
                      return r
                  S.add("pe", fnO, reads=Amk + qek + [("vtb", blk) for blk in range(4)] + [("Sbf", c) for c in range(8)],
                        writes=bk(bH))
                  if stop == 'B5a':
                      raise _Stop()
                  S.add("act", lambda e, bH=bH: e.activation(out=osq, in_=bank(bH), func=AF.Square), reads=bk(bH), writes=osqk)
                  if stop == 'B5b':
                      raise _Stop()
                  S.add("dve", lambda e, bH=bH: e.tensor_copy(out=T1, in_=bank(bH)), reads=bk(bH), writes=T1k)
                  bfree(bH)
                  if stop == 'B5':
                      raise _Stop()
                  bI = balloc()
                  S.add("pe", lambda e, bI=bI: e.matmul(bank(bI), lhsT=onesb, rhs=osq, start=True, stop=True),
                        reads=osqk + ["cmk"], writes=bk(bI))
                  S.add("act", lambda e, bI=bI: e.activation(out=T3, in_=bank(bI), func=AF.Ln, bias=epsc, scale=1.0 / 128),
                        reads=bk(bI) + ["cst"], writes=T3k)
                  bfree(bI)
                  S.add("act", lambda e: e.activation(out=T3, in_=T3, func=AF.Exp, scale=-0.5), reads=T3k, writes=T3k)
                  S.add("dve", lambda e: e.tensor_tensor(out=T1, in0=T1, in1=T3, op=ALU.mult), reads=T1k + T3k, writes=T1k)
                  S.add("dve", lambda e, h=h: e.scalar_tensor_tensor(out=ybT[:, h, :], in0=T1, scalar=cst[:, C_HGN:C_HGN + 1],
                                                                    in1=ybT[:, h, :], op0=ALU.mult, op1=ALU.mult),
                        reads=T1k + ["cst", ("ybT", h)], writes=[("ybT", h)])

              for h in range(4):
                  if h % 2 == 0:
                      hgrn_head(h, Rf(0, 2), Rk(0, 2), Rf(2, 2), Rk(2, 2), Rf(4, 2), Rk(4, 2), Rbf(6, 1), Rk(6, 1), Rbf(7, 1), Rk(7, 1),
                                ebe[:, 0:8], "ebe0")
                  else:
                      hgrn_head(h, Rf(18, 2), Rk(18, 2), Rf(20, 2), Rk(20, 2), Rf(22, 2), Rk(22, 2), Rbf(8, 1), Rk(8, 1), Rbf(11, 1), Rk(11, 1),
                                ebe[:, 8:16], "ebe1")

              w_release(3)
              if stop == 'B':
                  raise _Stop()
              S.tag = 't%d.%d:attn' % (s, ti)
              bpool[0] = (4, 5, 6, 7)
              cur4 = cmk[:, M_CUR:M_CUR + 512]
              prev4 = cmk[:, M_PREV:M_PREV + 512]
              c16 = cmk[:, M_C16 + 512 * j16:M_C16 + 512 * j16 + 512]
              p16 = cmk[:, M_P16 + 512 * j16:M_P16 + 512 * j16 + 512]
              nsb, nsbk = Rf(12, 2), Rk(12, 2)
              dnr, dnrk = Rf(14, 2), Rk(14, 2)
              for h8 in range(8):
                  hp, e8 = h8 // 2, h8 % 2
                  pb = 64 * e8
                  bO = balloc()
                  groups = []
                  qTh = (qTe if e8 == 0 else qTo)[:, hp, :]
                  kTh = kT[:, hp, :]
                  qkey = ("qTe", hp) if e8 == 0 else ("qTo", hp)
                  g = dict(M=128, items=[], mask=cur4, mshape=(4, 128), vkeys=[("v1", par)], kkeys=[("kT", hp, ti)])
                  for i in range(4):
                      g["items"].append((kTh[:, t0 + 128 * i:t0 + 128 * i + 128], qTh[:, 128 * i:128 * i + 128], i * 128, 128,
                                         v1[:, par, i, h8 * 65:h8 * 65 + 65], bank(bO)[0:65, 128 * i:128 * i + 128]))
                  groups.append(g)
                  g = dict(M=128, items=[], mask=prev4, mshape=(4, 128), vkeys=[("v1", par), ("v1", 1 - par)],
                           kkeys=[("kT", hp, ti)] + ([("kT", hp, ti - 1)] if ti > 0 else []))
                  for i in range(4):
                      if i == 0 and ti == 0:
                          continue
                      vsrc = v1[:, par, i - 1, h8 * 65:h8 * 65 + 65] if i > 0 else v1[:, 1 - par, 3, h8 * 65:h8 * 65 + 65]
                      g["items"].append((kTh[:, t0 + 128 * (i - 1):t0 + 128 * i], qTh[:, 128 * i:128 * i + 128], i * 128, 128,
                                         vsrc, bank(bO)[0:65, 128 * i:128 * i + 128]))
                  groups.append(g)
                  g = dict(M=128, items=[], mask=cur4, mshape=(4, 128), vkeys=[("v4", par)], kkeys=[("kT", hp, ti)])
                  for r in range(4):
                      g["items"].append((kTh[:, t0 + r:t0 + T:4], qTh[:, r:T:4], r * 128, 128,
                                         v4[:, par, r, h8 * 65:h8 * 65 + 65], bank(bO)[0:65, r:T:4]))
                  groups.append(g)
                  if ti > 0:
                      g = dict(M=128, items=[], mask=prev4, mshape=(4, 128), vkeys=[("v4", 1 - par)], kkeys=[("kT", hp, ti - 1)])
                      for r in range(4):
                          g["items"].append((kTh[:, t0 - T + r:t0:4], qTh[:, r:T:4], r * 128, 128,
                                             v4[:, 1 - par, r, h8 * 65:h8 * 65 + 65], bank(bO)[0:65, r:T:4]))
                      groups.append(g)
                  M16 = 128
                  base16 = 2048 * n16
                  g = dict(M=M16, items=[], mask=c16, mshape=(16, 32), vkeys=[("v16", n16 % 2, jj) for jj in range(j16 + 1)],
                           kkeys=[("kT", hp, 4 * n16 + jj) for jj in range(j16 + 1)])
                  for r in range(16):
                      g["items"].append((kTh[:, base16 + r:base16 + 2048:16], qTh[:, r:T:16], r * 32, 32,
                                         v16[0:M16, n16 % 2, r, h8 * 65:h8 * 65 + 65], bank(bO)[0:65, r:T:16]))
                  groups.append(g)
                  if n16 > 0:
                      g = dict(M=128, items=[], mask=p16, mshape=(16, 32), vkeys=[("v16", (n16 - 1) % 2, jj) for jj in range(4)],
                               kkeys=[("kT", hp, 4 * (n16 - 1) + jj) for jj in range(4)])
                      for r in range(16):
                          g["items"].append((kTh[:, base16 - 2048 + r:base16:16], qTh[:, r:T:16], r * 32, 32,
                                             v16[:, (n16 - 1) % 2, r, h8 * 65:h8 * 65 + 65], bank(bO)[0:65, r:T:16]))
                      groups.append(g)

                  ngr = len(groups)
                  for gi, g in enumerate(groups):
                      bS = balloc()
                      M = g["M"]
                      pslot = 16 + (pt_ctr[0] % 2)
                      pt_ctr[0] += 1
                      Pt, Ptk = Rbf(pslot, 1), Rk(pslot, 1)

                      def fnS(e, g=g, bS=bS):
                          r = None
                          for (ka, qa, c0, n, va, oa) in g["items"]:
                              r = e.matmul(bank(bS)[0:g["M"], c0:c0 + n], lhsT=ka, rhs=qa, start=True, stop=True)
                          return r
                      S.add("pe", fnS, reads=g["kkeys"] + [qkey], writes=bk(bS))
                      c_lo = min(it[2] for it in g["items"])
                      c_hi = max(it[2] + it[3] for it in g["items"])
                      S.add("act", lambda e, bS=bS, M=M, Pt=Pt, c_lo=c_lo, c_hi=c_hi: e.activation(
                          out=Pt[0:M, c_lo:c_hi], in_=bank(bS)[0:M, c_lo:c_hi], func=AF.Exp, scale=0.125),
                          reads=bk(bS), writes=Ptk)
                      bfree(bS)
                      a_, b_ = g["mshape"]
                      msk = g["mask"]
                      S.add("dve", lambda e, M=M, Pt=Pt, msk=msk, c_lo=c_lo, c_hi=c_hi: e.tensor_tensor(
                          out=Pt[0:M, c_lo:c_hi], in0=Pt[0:M, c_lo:c_hi], in1=msk[0:M, c_lo:c_hi], op=ALU.mult),
                          reads=Ptk + ["cmk"], writes=Ptk)

                      def fnP(e, g=g, Pt=Pt, first_group=(gi == 0), last_group=(gi == ngr - 1)):
                          r = None
                          nit = len(g["items"])
                          for ii, (ka, qa, c0, n, va, oa) in enumerate(g["items"]):
                              r = e.matmul(oa, lhsT=va, rhs=Pt[0:g["M"], c0:c0 + n], start=(first_group and ii == 0),
                                           stop=(last_group and ii == nit - 1))
                          return r
                      S.add("pe", fnP, reads=Ptk + g["vkeys"], writes=bk(bO))
                  S.add("act", lambda e, bO=bO: e.activation(out=dnr[64:65, :], in_=bank(bO)[64:65, :], func=AF.Ln), reads=bk(bO), writes=dnrk)
                  S.add("act", lambda e, bO=bO: e.activation(out=nsb[0:64, :], in_=bank(bO)[0:64, :], func=AF.Copy), reads=bk(bO), writes=nsbk)
                  bfree(bO)
                  bZ = balloc()
                  S.add("dve", lambda e: e.tensor_copy(out=hib[64:65, :], in_=dnr[64:65, :]), reads=dnrk, writes=["hib"])
                  S.add("dve", lambda e: e.tensor_tensor(out=lob[64:65, :], in0=dnr[64:65, :], in1=hib[64:65, :], op=ALU.subtract),
                        reads=dnrk + ["hib"], writes=["lob"])

                  def fnZ(e, bZ=bZ):
                      e.matmul(bank(bZ), lhsT=cmk[:, M_SEL:M_SEL + 128], rhs=hib[:], start=True, stop=False)
                      return e.matmul(bank(bZ), lhsT=cmk[:, M_SEL:M_SEL + 128], rhs=lob[:], start=False, stop=True)
                  S.add("pe", fnZ, reads=["hib", "lob", "cmk"], writes=bk(bZ))
                  S.add("act", lambda e, bZ=bZ: e.activation(out=dnr[0:64, :], in_=bank(bZ)[0:64, :], func=AF.Exp, scale=-1.0),
                        reads=bk(bZ), writes=dnrk)
                  S.add("dve", lambda e, pb=pb, hp=hp: e.tensor_tensor(out=yaT[pb:pb + 64, hp, :], in0=nsb[0:64, :],
                                                                      in1=dnr[0:64, :], op=ALU.mult),
                        reads=dnrk + nsbk, writes=[("yaT", h8)])
                  bfree(bZ)

              if stop == 'C':
                  raise _Stop()
              S.tag = 't%d.%d:merge' % (s, ti)
              bpool[0] = None
              mT, mTk = Rbf(0, 8).rearrange("p (c t) -> p c t", t=T), (lambda c: Rk(c, 1))
              Ta, Tak = Rf(8, 2), Rk(8, 2)
              Tb, Tbk = Rf(10, 2), Rk(10, 2)
              for c in range(8):
                  wm_, wmk = w_next(U_MG + c)
                  wa = wm_[:, 0:512].rearrange("p (k m) -> p k m", m=128)
                  wb = wm_[:, 512:1024].rearrange("p (k m) -> p k m", m=128)
                  wga = wm_[:, 1024:2048].rearrange("p (k m) -> p k m", m=128)
                  wgb = wm_[:, 2048:3072].rearrange("p (k m) -> p k m", m=128)
                  b1, b2, b3, b4 = balloc(), balloc(), balloc(), balloc()
                  dense_T(b1, lambda k, wa=wa: wa[:, k, :], 4, lambda k: yaT[:, k, :], lambda k: [("yaT", 2 * k), ("yaT", 2 * k + 1)], wmk)
                  dense_T(b2, lambda k, wga=wga: wga[:, k, :], 8, lambda k: hT[:, k, :], hkeys, wmk)
                  dense_T(b3, lambda k, wb=wb: wb[:, k, :], 4, lambda k: ybT[:, k, :], lambda k: [("ybT", k)], wmk)
                  dense_T(b4, lambda k, wgb=wgb: wgb[:, k, :], 8, lambda k: hT[:, k, :], hkeys, wmk)
                  S.add("act", lambda e, b2=b2: e.activation(out=Ta, in_=bank(b2), func=AF.Sigmoid), reads=bk(b2), writes=Tak)
                  S.add("act", lambda e, b4=b4: e.activation(out=Tb, in_=bank(b4), func=AF.Sigmoid), reads=bk(b4), writes=Tbk)
                  S.add("dve", lambda e, b1=b1: e.tensor_tensor(out=Ta, in0=bank(b1), in1=Ta, op=ALU.mult), reads=bk(b1) + Tak, writes=Tak)
                  S.add("dve", lambda e, b3=b3: e.tensor_tensor(out=Tb, in0=bank(b3), in1=Tb, op=ALU.mult), reads=bk(b3) + Tbk, writes=Tbk)
                  S.add("pool", lambda e, c=c, mT=mT: e.tensor_tensor(out=mT[:, c, :], in0=Ta, in1=Tb, op=ALU.add),
                        reads=Tak + Tbk, writes=mTk(c))
                  for b in (b1, b2, b3, b4):
                      bfree(b)
                  w_release()
              for u in range(2):
                  wo_, wok = w_next(U_OUT + u)
                  wov = wo_.rearrange("p (k c) -> p k c", c=512)
                  for cc in range(4):
                      c = 4 * u + cc
                      b = balloc()
                      dense_T(b, lambda k, cc=cc, wov=wov: wov[:, k, cc * 128:(cc + 1) * 128], 8, lambda k, mT=mT: mT[:, k, :],
                              lambda k: Rk(k, 1), wok)
                      S.add("dve", lambda e, c=c, b=b: e.tensor_tensor(out=xT[:, c, :], in0=bank(b), in1=xT[:, c, :], op=ALU.add),
                            reads=bk(b) + [("xT", c)], writes=[("xT", c)])
                      bfree(b)
                  w_release()

              if stop == 'D':
                  raise _Stop()
              S.tag = 't%d.%d:ffn' % (s, ti)
              rmsnorm(1, lambda c: hT[:, c, :], lambda c: [("hT", c)])
              hid = Rbf(0, 22).rearrange("p (c t) -> p c t", t=T)
              for i2 in range(11):
                  wg_, wgk = w_next(U_GU + i2)
                  wgv = wg_.rearrange("p (k g c) -> p k g c", g=2, c=256)
                  for ih in range(2):
                      i = 2 * i2 + ih
                      bg, bu = balloc(), balloc()
                      dense_T(bg, lambda k, ih=ih, wgv=wgv: wgv[:, k, 0, ih * 128:(ih + 1) * 128], 8, lambda k: hT[:, k, :], hkeys, wgk, split=(i == 0))
                      dense_T(bu, lambda k, ih=ih, wgv=wgv: wgv[:, k, 1, ih * 128:(ih + 1) * 128], 8, lambda k: hT[:, k, :], hkeys, wgk)
                      Tg, Tgk = Rf(22, 2), Rk(22, 2)
                      S.add("act", lambda e, bg=bg, Tg=Tg: e.activation(out=Tg, in_=bank(bg), func=AF.Silu), reads=bk(bg), writes=Tgk)
                      S.add("dve", lambda e, bu=bu, i=i, hid=hid, Tg=Tg: e.tensor_tensor(out=hid[:, i, :], in0=bank(bu), in1=Tg, op=ALU.mult),
                            reads=bk(bu) + Tgk, writes=Rk(i, 1))
                      bfree(bg)
                      bfree(bu)
                  w_release()
              for c in range(8):
                  wd_, wdk = w_next(U_DN + c)
                  wdv = wd_[:, 0:2816].rearrange("p (k m) -> p k m", m=128)
                  b = balloc()
                  dense_T(b, lambda k, wdv=wdv: wdv[:, k, :], 22, lambda k, hid=hid: hid[:, k, :], lambda k: Rk(k, 1), wdk)
                  S.add("dve", lambda e, c=c, b=b: e.tensor_tensor(out=xT[:, c, :], in0=bank(b), in1=xT[:, c, :], op=ALU.add),
                        reads=bk(b) + [("xT", c)], writes=[("xT", c)])
                  bfree(b)
                  w_release()

              if stop == 'E':
                  raise _Stop()
              S.tag = 't%d.%d:ple' % (s, ti)
              rmsnorm(2, lambda c: hT[:, c, :], lambda c: [("hT", c)])
              Tp, Tpk = Rf(0, 2), Rk(0, 2)
              pgu = []
              for u in range(2):
                  pgu.append(w_next(U_PG + u))
              wpe_, wpek = w_next(U_PE)
              wpev = wpe_[:, 0:2048].rearrange("p (k c) -> p k c", c=1024)
              for c in range(8):
                  wpg_, wpgk = pgu[c // 4]
                  wpgv = wpg_.rearrange("p (k c) -> p k c", c=512)
                  cc = c % 4
                  bp, be_ = balloc(), balloc()
                  dense_T(bp, lambda k, cc=cc, wpgv=wpgv: wpgv[:, k, cc * 128:(cc + 1) * 128], 8, lambda k: hT[:, k, :], hkeys, wpgk, split=(c == 0))
                  dense_T(be_, lambda k, c=c, wpev=wpev: wpev[:, k, c * 128:(c + 1) * 128], 2, lambda k: pT[:, k, :], lambda k: [("pT", k)], wpek)
                  S.add("act", lambda e, bp=bp: e.activation(out=Tp, in_=bank(bp), func=AF.Sigmoid), reads=bk(bp), writes=Tpk)
                  S.add("dve", lambda e, be_=be_: e.tensor_tensor(out=Tp, in0=bank(be_), in1=Tp, op=ALU.mult), reads=bk(be_) + Tpk, writes=Tpk)
                  S.add("dve", lambda e, c=c: e.tensor_tensor(out=xT[:, c, :], in0=xT[:, c, :], in1=Tp, op=ALU.add),
                        reads=Tpk + [("xT", c)], writes=[("xT", c)])
                  bfree(bp)
                  bfree(be_)
              w_release(3)

              if stop == 'F':
                  raise _Stop()
              S.tag = 't%d.%d:final' % (s, ti)
              rmsnorm(3, lambda c: xT[:, c, :], lambda c: [("xT", c)])
              for blk in range(4):
                  osl = 4 + 4 * (blk % 2)
                  otm, otmk = Rf(osl, 4), Rk(osl, 4)
                  for half in range(2):
                      b = balloc()

                      def fnF(e, b=b, blk=blk, half=half):
                          r = None
                          for cc in range(4):
                              r = e.transpose(bank(b)[:, cc * 128:(cc + 1) * 128], xT[:, half * 4 + cc, blk * 128:(blk + 1) * 128], ident)
                          return r
                      S.add("pe", fnF, reads=[("xT", half * 4 + cc) for cc in range(4)] + ["cst"], writes=bk(b))
                      if half == 0:
                          S.add("act", lambda e, b=b, otm=otm: e.activation(out=otm[:, 0:512], in_=bank(b), func=AF.Copy),
                                reads=bk(b), writes=Rk(osl, 2))
                      else:
                          S.add("dve", lambda e, b=b, otm=otm: e.tensor_copy(out=otm[:, 512:1024], in_=bank(b)),
                                reads=bk(b), writes=Rk(osl + 2, 2))
                      bfree(b)
                  S.add("sp", lambda e, otm=otm, blk=blk, row0=row0: e.dma_start(out=out_d[row0 + 128 * blk:row0 + 128 * (blk + 1), :], in_=otm),
                        reads=otmk, writes=[("outd", s, ti, blk)], dkey=("out", blk % 2))

    except _Stop:
        pass
    assert stop is not None or wstate["consumed"] == len(unit_seq)
    S.emit(nc, stack)
    stack.close()
    return nc, S


def _pack_weights(w_in, w_a_up, w_b_up, w_out, w_gu, w_down, w_pe, w_pg):
    wp = np.zeros((NU, 128, USZ), np.float32)

    def kc(w, c0, c1):
        nk = w.shape[0] // 128
        return w[:, c0:c1].reshape(nk, 128, c1 - c0).transpose(1, 0, 2)

    for u in range(11):
        wp[U_IN + u] = kc(w_in, 512 * u, 512 * u + 512).reshape(128, -1)
    for c in range(8):
        wp[U_MG + c, :, 0:512] = kc(w_a_up, 128 * c, 128 * c + 128).reshape(128, -1)
        wp[U_MG + c, :, 512:1024] = kc(w_b_up, 128 * c, 128 * c + 128).reshape(128, -1)
        wp[U_MG + c, :, 1024:2048] = kc(w_in, 3584 + 128 * c, 3584 + 128 * c + 128).reshape(128, -1)
        wp[U_MG + c, :, 2048:3072] = kc(w_in, 4608 + 128 * c, 4608 + 128 * c + 128).reshape(128, -1)
    for u in range(2):
        wp[U_OUT + u] = kc(w_out, 512 * u, 512 * u + 512).reshape(128, -1)
        wp[U_PG + u] = kc(w_pg, 512 * u, 512 * u + 512).reshape(128, -1)
    for i2 in range(11):
        g = kc(w_gu, 256 * i2, 256 * i2 + 256)
        uu = kc(w_gu, FFN + 256 * i2, FFN + 256 * i2 + 256)
        wp[U_GU + i2] = np.stack([g, uu], axis=2).reshape(128, -1)
    for c in range(8):
        wp[U_DN + c, :, 0:2816] = kc(w_down, 128 * c, 128 * c + 128).reshape(128, -1)
    wp[U_PE, :, 0:2048] = kc(w_pe, 0, 1024).reshape(128, -1)
    return wp


def _consts(norm_mix, norm_ffn, norm_ple, norm_final, hg_norm, hg_lb):
    cst = np.zeros((128, NCF), np.float32)
    cst[:, C_ID:C_ID + 128] = np.eye(128, dtype=np.float32)
    for gi, g in enumerate((norm_mix, norm_ffn, norm_ple, norm_final)):
        cst[:, C_G + 8 * gi:C_G + 8 * gi + 8] = np.asarray(g, np.float32).reshape(8, 128).T
    cst[:, C_HGN] = np.asarray(hg_norm, np.float32).reshape(128)
    cst[:, C_LB:C_LB + 8] = np.asarray(hg_lb, np.float32).reshape(2, 4, 128).transpose(2, 0, 1).reshape(128, 8)
    cst[:, C_EPS] = EPS
    mk = np.zeros((128, NMK), np.float32)
    kk = np.arange(128)[:, None]
    qq = np.arange(128)[None, :]
    mk[:, M_CUR:M_CUR + 512] = np.tile((kk <= qq), (1, 4))
    mk[:, M_PREV:M_PREV + 512] = np.tile((kk >= qq), (1, 4))
    mk[:, M_HG:M_HG + 512] = np.tile((kk <= qq) & ((kk // 64) == (qq // 64)), (1, 4))
    a2 = np.arange(32)[None, :]
    for j in range(4):
        mk[:, M_C16 + 512 * j:M_C16 + 512 * j + 512] = np.tile((kk <= 32 * j + a2), (1, 16))
        mk[:, M_P16 + 512 * j:M_P16 + 512 * j + 512] = np.tile((kk >= 32 * j + a2), (1, 16))
    mk[:, M_ONE:M_ONE + 128] = 1.0
    mk[64, M_SEL:M_SEL + 128] = 1.0
    seg = np.ones(512, np.float32)
    seg[0::64] = 0.0
    mk[:, M_SEG:M_SEG + 512] = seg[None, :]
    return cst, mk


_CACHE = {}


def kernel(x, p, norm_mix, w_in, hg_lb, hg_norm, w_a_up, w_b_up, w_out, norm_ffn,
           w_gu, w_down, norm_ple, w_pe, w_pg, norm_final):
    f = lambda a: np.ascontiguousarray(np.asarray(a, dtype=np.float32))
    x = f(x)
    p = f(p)
    wp = _pack_weights(f(w_in)[0], f(w_a_up)[0], f(w_b_up)[0], f(w_out)[0], f(w_gu)[0], f(w_down)[0], f(w_pe)[0], f(w_pg)[0])
    cst, mk = _consts(f(norm_mix)[0], f(norm_ffn)[0], f(norm_ple)[0], f(norm_final), f(hg_norm)[0], f(hg_lb))
    if "nc" not in _CACHE:
        _CACHE["nc"] = build_program()[0]
    nc = _CACHE["nc"]
    in_maps = []
    for c in range(NCORES):
        in_maps.append({
            "x": x[NSEQ * c:NSEQ * (c + 1)].reshape(NSEQ * SEQ, D),
            "p": p[0, NSEQ * c:NSEQ * (c + 1)].reshape(NSEQ * SEQ, 256),
            "wpack": wp, "cst": cst, "cmask": mk,
        })
    res = run_bass_kernel_spmd(nc, in_maps, core_ids=list(range(NCORES)))
    out = np.concatenate([np.asarray(r["out"]).reshape(NSEQ, SEQ, D) for r in res.results], axis=0)
    return out.astype(np.float32)
```

```python
from contextlib import ExitStack

import numpy as np
import concourse.bass as bass
import concourse.mybir as mybir
from concourse.bass_utils import run_bass_kernel_spmd

F32 = mybir.dt.float32
BF16 = mybir.dt.bfloat16
AF = mybir.ActivationFunctionType
ALU = mybir.AluOpType

NCORES = 8
SEQ = 4096
D = 1024
T = 512
NT = SEQ // T
NSEQ = 2
FFN = 2816
EPS = 1e-6
VW = 520
RING = 4
USZ = 4096

C_ID = 0
C_G = 128
C_HGN = 160
C_LB = 161
C_EPS = 169
NCF = 170
M_CUR = 0
M_PREV = 512
M_HG = 1024
M_C16 = 1536
M_P16 = 3584
M_ONE = 5632
M_SEL = 5760
M_SEG = 5888
NMK = 6400

U_IN = 0
U_MG = 11
U_OUT = 19
U_GU = 21
U_DN = 32
U_PG = 40
U_PE = 42
NU = 43


class _RecInst:
    def then_inc(self, *a, **k):
        return self


class _Rec:
    def __init__(self):
        self.calls = []

    def __getattr__(self, name):
        def f(*a, **kw):
            self.calls.append((name, a, kw))
            return _RecInst()
        return f


def _fsz(ap):
    n = 1
    for d in tuple(ap.shape)[1:]:
        n *= int(d)
    return n


def _est(eng, calls):
    t = 0.0
    lat = 0.0
    for name, a, kw in calls:
        if name == "matmul":
            rhs = kw.get("rhs")
            n = _fsz(rhs)
            mult = 4.0 if rhs.dtype == F32 else 1.0
            if n >= 512:
                t += n * mult / 1.95 + 16.0
            else:
                t += max(64, n) * mult / 1.95 + 46.0
        elif name == "transpose":
            t += 128 * 2.0 / 1.95 + 16.0
        elif name == "dma_start":
            out = kw.get("out")
            nbytes = 4 * int(out.shape[0]) * _fsz(out)
            t += 1500.0 if eng == "pool" else 150.0
            lat = max(lat, 2500.0 + nbytes / 120.0)
        elif name == "activation":
            t += max(64, _fsz(kw.get("in_"))) * 0.83 + 220.0
        elif name == "tensor_tensor_scan":
            t += 2 * _fsz(kw.get("data0")) * 1.04 + 200.0
        else:
            ap = kw.get("out")
            if ap is None:
                ap = a[0]
            n = _fsz(ap)
            if eng == "pool":
                t += n * 1.7 + 350.0
            else:
                t += max(64, n) * 1.04 + 200.0
    return t, lat


class Sched:
    SYNC = 250.0

    def schedule(self, engs):
        ops = self.ops
        n = len(ops)
        cost = [0.0] * n
        lat = [0.0] * n
        for i, o in enumerate(ops):
            rec = _Rec()
            o["fn"](rec)
            cost[i], lat[i] = _est(o["eng"], rec.calls)
        dependents = [[] for _ in range(n)]
        ndeps = [0] * n
        for i, o in enumerate(ops):
            for j in o["deps"]:
                dependents[j].append(i)
            ndeps[i] = len(o["deps"])
        ready = [0.0] * n
        avail = {e: [] for e in engs}
        for i in range(n):
            if ndeps[i] == 0:
                avail[ops[i]["eng"]].append(i)
        free = {e: 0.0 for e in engs}
        order = {e: [] for e in engs}
        busy = {e: 0.0 for e in engs}
        done = 0
        tend = 0.0
        while done < n:
            best = None
            for e in engs:
                av = avail[e]
                if not av:
                    continue
                t = free[e]
                bk_ = None
                bi = -1
                for i in av:
                    r = ready[i]
                    k_ = (r if r > t else t, i)
                    if bk_ is None or k_ < bk_:
                        bk_ = k_
                        bi = i
                if best is None or bk_ < best[0]:
                    best = (bk_, bi, e)
            (st, _), i, e = best
            avail[e].remove(i)
            ops[i]["st"] = st
            ops[i]["cost"] = cost[i]
            fin = st + cost[i]
            free[e] = fin
            busy[e] += cost[i]
            order[e].append(i)
            fin += lat[i]
            ops[i]["fin"] = fin
            tend = max(tend, fin)
            for d in dependents[i]:
                if fin + self.SYNC > ready[d]:
                    ready[d] = fin + self.SYNC
                ndeps[d] -= 1
                if ndeps[d] == 0:
                    avail[ops[d]["eng"]].append(d)
            done += 1
        self.model_ns = tend
        self.model_busy = busy
        return order

    def __init__(self):
        self.ops = []
        self.last_w = {}
        self.readers = {}

    def add(self, eng, fn, reads=(), writes=(), dkey=None, ndma=1):
        i = len(self.ops)
        deps = set()
        writes = list(writes) + [k for k in reads if isinstance(k, tuple) and k[0] == "ps"]
        for k in reads:
            if k in self.last_w:
                deps.add(self.last_w[k])
        for k in writes:
            if k in self.last_w:
                deps.add(self.last_w[k])
            deps.update(self.readers.get(k, ()))
        for k in reads:
            self.readers.setdefault(k, []).append(i)
        for k in writes:
            self.last_w[k] = i
            self.readers[k] = []
        deps.discard(i)
        self.ops.append(dict(eng=eng, fn=fn, deps=deps, dkey=dkey, ndma=ndma, tag=getattr(self, "tag", "")))
        return i

    def emit(self, nc, stack, final_engine="sp", reorder=True):
        ops = self.ops
        engs = ["pe", "act", "dve", "pool", "sp"]
        if reorder:
            per_eng = self.schedule(engs)
        else:
            per_eng = {e: [] for e in engs}
            for i, o in enumerate(ops):
                per_eng[o["eng"]].append(i)
        needed = set()
        for i, o in enumerate(ops):
            for j in o["deps"]:
                oj = ops[j]
                if oj["dkey"] is None and oj["eng"] == "pe" and o["eng"] == "pe" and o["dkey"] is None:
                    continue
                needed.add(j)
        esem = {e: stack.enter_context(nc.semaphore("s_" + e)) for e in engs}
        dsem = {}
        dtot = {}
        cnt = {e: 0 for e in engs}
        sig = {}
        out_waits = []
        for i, o in enumerate(ops):
            if o["dkey"] is not None:
                k = o["dkey"]
                if k not in dsem:
                    dsem[k] = stack.enter_context(nc.semaphore("d_%d" % len(dsem)))
                    dtot[k] = 0
                dtot[k] += 16 * o["ndma"]
                sig[i] = (dsem[k], dtot[k])
        for e in engs:
            for i in per_eng[e]:
                if ops[i]["dkey"] is None and i in needed:
                    cnt[e] += 1
                    sig[i] = (esem[e], cnt[e])
        self.nsem = 5 + len(dsem)
        self.counts = dict(cnt)
        final = {}
        for k in dsem:
            if isinstance(k, tuple) and k[0] == "out":
                final[k] = (dsem[k], dtot[k])

        def run_engine(ename, eobj):
            waited = {}
            for i in per_eng[ename]:
                o = ops[i]
                w = {}
                for j in o["deps"]:
                    if j not in sig:
                        continue
                    oj = ops[j]
                    if oj["dkey"] is None and oj["eng"] == "pe" and ename == "pe" and o["dkey"] is None:
                        continue
                    s, v = sig[j]
                    if w.get(s.num, (None, 0))[1] < v:
                        w[s.num] = (s, v)
                for num, (s, v) in w.items():
                    if waited.get(num, 0) >= v:
                        continue
                    eobj.wait_ge(s, v)
                    waited[num] = v
                r = o["fn"](eobj)
                if o["dkey"] is not None:
                    insts = r if isinstance(r, (list, tuple)) else [r]
                    assert len(insts) == o["ndma"], (len(insts), o["ndma"])
                    for ins in insts:
                        ins.then_inc(sig[i][0], 16)
                elif i in sig:
                    r.then_inc(sig[i][0], 1)
            if ename == final_engine:
                for k, (s, v) in final.items():
                    eobj.wait_ge(s, v)

        block = stack.enter_context(nc.Block())

        @block.tensor
        def _(e):
            run_engine("pe", e)

        @block.scalar
        def _(e):
            run_engine("act", e)

        @block.vector
        def _(e):
            run_engine("dve", e)

        @block.gpsimd
        def _(e):
            run_engine("pool", e)

        @block.sync
        def _(e):
            run_engine("sp", e)


class _Stop(Exception):
    pass


def build_program(nseq=NSEQ, ntiles=NT, stop=None):
    nc = bass.Bass("TRN2", target_bir_lowering=False)
    x_d = nc.dram_tensor("x", [NSEQ * SEQ, D], F32, kind="ExternalInput").ap()
    p_d = nc.dram_tensor("p", [NSEQ * SEQ, 256], F32, kind="ExternalInput").ap()
    wp_d = nc.dram_tensor("wpack", [NU, 128, USZ], F32, kind="ExternalInput").ap()
    cst_d = nc.dram_tensor("cst", [128, NCF], F32, kind="ExternalInput").ap()
    cmk_d = nc.dram_tensor("cmask", [128, NMK], F32, kind="ExternalInput").ap()
    out_d = nc.dram_tensor("out", [NSEQ * SEQ, D], F32, kind="ExternalOutput").ap()
    vscr = nc.dram_tensor("vscr", [NSEQ, SEQ, VW], BF16, kind="Internal").ap()

    S = Sched()
    stack = ExitStack()

    def sb(name, shape, dt):
        return stack.enter_context(nc.sbuf_tensor("sb_" + name, shape, dt))

    cst = sb("cst", [128, NCF], F32)
    cmk = sb("cmk", [128, NMK], BF16)
    lbc = sb("lbc", [128, 8], F32)
    kT = sb("kT", [128, 4, SEQ], BF16)
    v16 = sb("v16", [128, 2, 16, VW], BF16)
    v4 = sb("v4", [128, 2, 4, VW], BF16)
    v1 = sb("v1", [128, 2, 4, VW], BF16)
    wr = sb("wr", [128, RING, USZ], BF16)
    xT = sb("xT", [128, 8, T], F32)
    pT = sb("pT", [128, 2, T], BF16)
    hT = sb("hT", [128, 8, T], BF16)
    qTe = sb("qTe", [128, 4, T], BF16)
    qTo = sb("qTo", [128, 4, T], BF16)
    ktmA = sb("ktmA", [128, 4, 128], BF16)
    ktmB = sb("ktmB", [128, 4, 128], BF16)
    hib = sb("hib", [128, T], BF16)
    lob = sb("lob", [128, T], BF16)
    yaT = sb("yaT", [128, 4, T], BF16)
    ybT = sb("ybT", [128, 4, T], BF16)
    vtb = sb("vtb", [128, 4, 512], BF16)
    St = sb("St", [128, 4, 128], F32)
    Sbf = sb("Sbf", [128, 8, 128], BF16)
    ebe = sb("ebe", [128, 16], F32)
    sq = sb("sq", [128, 2, T], BF16)
    R = sb("R", [128, 12288], BF16)
    ps = stack.enter_context(nc.psum_tensor("psum_all", [128, 8, 512], F32))

    def Rk(s0, n):
        return [("R", s) for s in range(s0, s0 + n)]

    def Rbf(s0, n):
        return R[:, s0 * 512:(s0 + n) * 512]

    def Rf(s0, n):
        return R[:, s0 * 512:(s0 + n) * 512].bitcast(F32)

    rstd = Rf(22, 2)
    rstdk = Rk(22, 2)

    free_banks = list(range(8))

    bpool = [None]
    pt_ctr = [0]

    def balloc():
        allowed = bpool[0]
        for idx, b in enumerate(free_banks):
            if allowed is None or b in allowed:
                return free_banks.pop(idx)
        raise AssertionError("out of PSUM banks")

    def bfree(b):
        free_banks.append(b)

    def bank(b):
        return ps[:, b, :]

    def bk(b):
        return [("ps", b)]

    ident = cst[:, C_ID:C_ID + 128]
    epsc = cst[:, C_EPS:C_EPS + 1]
    segm = cmk[:, M_SEG:M_SEG + 512]
    onesb = cmk[:, M_ONE:M_ONE + 128]

    S.add("sp", lambda e: e.dma_start(out=cst[:], in_=cst_d), writes=["cst"], dkey=("c", 0))
    S.add("pool", lambda e: e.dma_start(out=cmk[:], in_=cmk_d), writes=["cmk"], dkey=("c", 1))
    S.add("dve", lambda e: e.tensor_tensor(out=lbc[:, 0:4], in0=cst[:, C_LB:C_LB + 4], in1=cst[:, C_LB + 4:C_LB + 8],
                                           op=ALU.subtract), reads=["cst"], writes=["lbc"])
    S.add("act", lambda e: e.activation(out=lbc[:, 0:4], in_=lbc[:, 0:4], func=AF.Sigmoid), reads=["lbc"], writes=["lbc"])
    S.add("dve", lambda e: e.tensor_scalar(out=lbc[:, 4:8], in0=lbc[:, 0:4], scalar1=-1.0, scalar2=1.0,
                                           op0=ALU.mult, op1=ALU.add), reads=["lbc"], writes=["lbc2"])
    v1v = v1[:].rearrange("p a b (h c) -> p (a b h) c", c=65)
    S.add("pool", lambda e: e.memset(v1v[:, :, 64:65], 1.0), writes=[("v1", 0), ("v1", 1)])
    S.add("pool", lambda e: e.memset(qTe[64:128, :, :], 0.0), writes=["qTe_z"])
    S.add("pool", lambda e: e.memset(qTo[0:64, :, :], 0.0), writes=["qTo_z"])
    S.add("pool", lambda e: e.memset(ktmA[64:128, :, :], 0.0), writes=["ktmA_z"])
    S.add("pool", lambda e: e.memset(ktmB[0:64, :, :], 0.0), writes=["ktmB_z"])
    S.add("dve", lambda e: e.memset(hib[:], 0.0), writes=["hib"])
    S.add("dve", lambda e: e.memset(lob[:], 0.0), writes=["lob"])
    for hp_ in range(4):
        S.add("pool", lambda e, hp_=hp_: e.memset(kT[:, hp_, :], 0.0), writes=[("kT", hp_, tt) for tt in range(NT)])
    for pp_ in range(2):
        S.add("dve", lambda e, pp_=pp_: e.memset(v16[:, pp_, :, :], 0.0), writes=[("v16", pp_, jj) for jj in range(4)])

    unit_seq = []
    per_tile_units = (
        [(U_IN + u, 4096) for u in (0, 1, 2, 5, 3, 4, 6)]
        + [(U_MG + c, 3072) for c in range(8)]
        + [(U_OUT + u, 4096) for u in range(2)]
        + [(U_GU + u, 4096) for u in range(11)]
        + [(U_DN + c, 2816) for c in range(8)]
        + [(U_PG + u, 4096) for u in range(2)]
        + [(U_PE, 2048)]
    )
    for _ in range(nseq * ntiles):
        unit_seq.extend(per_tile_units)
    wstate = dict(issued=0, consumed=0, released=0)

    def w_issue():
        i = wstate["issued"]
        if i >= len(unit_seq):
            return
        uid, n = unit_seq[i]
        slot = i % RING
        S.add("pool", (lambda e, slot=slot, uid=uid, n=n: e.dma_start(out=wr[:, slot, 0:n], in_=wp_d[uid, :, 0:n])),
              writes=[("w", slot)], dkey=("w", slot))
        wstate["issued"] += 1

    def w_next(expect_uid):
        i = wstate["consumed"]
        uid, n = unit_seq[i]
        assert uid == expect_uid, (uid, expect_uid)
        assert wstate["issued"] > i and i - wstate["released"] < RING
        wstate["consumed"] += 1
        slot = i % RING
        return wr[:, slot, :], [("w", slot)]

    def w_release(k=1):
        for _ in range(k):
            assert wstate["released"] < wstate["consumed"]
            wstate["released"] += 1
            w_issue()

    for _ in range(RING):
        w_issue()

    def rmsnorm(gidx, out_fn, out_keys_fn):
        b = balloc()
        for c in range(8):
            sl = c % 2
            S.add("act", lambda e, c=c, sl=sl: e.activation(out=sq[:, sl, :], in_=xT[:, c, :], func=AF.Square),
                  reads=[("xT", c)], writes=[("sq", sl)])
            S.add("pe", lambda e, c=c, sl=sl: e.matmul(bank(b), lhsT=onesb, rhs=sq[:, sl, :], start=(c == 0), stop=(c == 7)),
                  reads=[("sq", sl), "cmk"], writes=bk(b))
        S.add("act", lambda e: e.activation(out=rstd[:], in_=bank(b), func=AF.Ln, bias=epsc, scale=1.0 / D),
              reads=bk(b) + ["cst"], writes=rstdk)
        bfree(b)
        S.add("act", lambda e: e.activation(out=rstd[:], in_=rstd[:], func=AF.Exp, scale=-0.5),
              reads=rstdk, writes=rstdk)
        for c in range(8):
            S.add("dve", lambda e, c=c: e.scalar_tensor_tensor(out=out_fn(c), in0=xT[:, c, :],
                                                              scalar=cst[:, C_G + gidx * 8 + c:C_G + gidx * 8 + c + 1],
                                                              in1=rstd[:], op0=ALU.mult, op1=ALU.mult),
                  reads=[("xT", c), "cst"] + rstdk, writes=out_keys_fn(c))

    def dense_T(b, lhs_fn, nk, rhs_fn, rkeys_fn, wkeys, N=T, split=False):
        if split:
            for k in range(nk):
                S.add("pe", (lambda e, k=k: e.matmul(bank(b)[:, 0:N], lhsT=lhs_fn(k), rhs=rhs_fn(k), start=(k == 0),
                                                     stop=(k == nk - 1))), reads=rkeys_fn(k) + wkeys, writes=bk(b))
            return

        def fn(e):
            r = None
            for k in range(nk):
                r = e.matmul(bank(b)[:, 0:N], lhsT=lhs_fn(k), rhs=rhs_fn(k), start=(k == 0), stop=(k == nk - 1))
            return r
        rk = []
        for k in range(nk):
            rk += rkeys_fn(k)
        S.add("pe", fn, reads=rk + wkeys, writes=bk(b))

    hkeys = lambda k: [("hT", k)]

    try:
      for s in range(nseq):
          for ti in range(ntiles):
              t0 = ti * T
              row0 = s * SEQ + t0
              par = ti % 2
              n16 = ti // 4
              j16 = ti % 4

              S.tag = 't%d.%d:load' % (s, ti)
              bpool[0] = (0, 1, 2, 3)
              xin = Rf(0, 16).rearrange("p (b d) -> p b d", d=D)
              pin = Rf(16, 4).rearrange("p (b d) -> p b d", d=256)
              for blk in range(4):
                  S.add("sp", lambda e, xin=xin, row0=row0, blk=blk: e.dma_start(
                      out=xin[:, blk, :], in_=x_d[row0 + 128 * blk:row0 + 128 * (blk + 1), :]),
                      writes=Rk(4 * blk, 4), dkey=("xin", blk))
              S.add("sp", lambda e, pin=pin, row0=row0: e.dma_start(
                  out=pin, in_=p_d[row0:row0 + T, :].rearrange("(b p) d -> p b d", p=128)),
                  writes=Rk(16, 4), dkey=("pin", 0))
              for blk in range(4):
                  for hh in range(2):
                      b = balloc()

                      def fn(e, hh=hh, b=b, xin=xin, blk=blk):
                          r = None
                          for cc in range(4):
                              c = 4 * hh + cc
                              r = e.transpose(bank(b)[:, cc * 128:(cc + 1) * 128], xin[:, blk, c * 128:(c + 1) * 128], ident)
                          return r
                      S.add("pe", fn, reads=Rk(4 * blk, 4) + ["cst"], writes=bk(b))
                      dst = xT[:, 4 * hh:4 * hh + 4, blk * 128:(blk + 1) * 128]
                      src = bank(b).rearrange("p (c t) -> p c t", t=128)
                      if hh == 0:
                          S.add("act", lambda e, dst=dst, src=src: e.activation(out=dst, in_=src, func=AF.Copy),
                                reads=bk(b), writes=[("xT", 4 * hh + cc) for cc in range(4)])
                      else:
                          S.add("dve", lambda e, dst=dst, src=src: e.tensor_copy(out=dst, in_=src),
                                reads=bk(b), writes=[("xT", 4 * hh + cc) for cc in range(4)])
                      bfree(b)
              for c in range(2):
                  b = balloc()

                  def fn(e, c=c, b=b, pin=pin):
                      r = None
                      for blk in range(4):
                          r = e.transpose(bank(b)[:, blk * 128:(blk + 1) * 128], pin[:, blk, c * 128:(c + 1) * 128], ident)
                      return r
                  S.add("pe", fn, reads=Rk(16, 4) + ["cst"], writes=bk(b))
                  S.add("dve", lambda e, c=c, b=b: e.tensor_copy(out=pT[:, c, :], in_=bank(b)),
                        reads=bk(b), writes=[("pT", c)])
                  bfree(b)

              S.tag = 't%d.%d:proj' % (s, ti)
              rmsnorm(0, lambda c: hT[:, c, :], lambda c: [("hT", c)])

              wq, wqk = w_next(U_IN + 0)
              wqv = wq.rearrange("p (k c) -> p k c", c=512)
              for hp in range(4):
                  b = balloc()
                  dense_T(b, lambda k, hp=hp, wqv=wqv: wqv[:, k, hp * 128:(hp + 1) * 128], 8, lambda k: hT[:, k, :], hkeys, wqk, split=(hp == 0))
                  S.add("act", lambda e, hp=hp, b=b: e.activation(out=qTe[0:64, hp, :], in_=bank(b)[0:64, :], func=AF.Copy),
                        reads=bk(b) + ["qTe_z"], writes=[("qTe", hp)])
                  S.add("dve", lambda e, hp=hp, b=b: e.tensor_copy(out=qTo[64:128, hp, :], in_=bank(b)[64:128, :]),
                        reads=bk(b) + ["qTo_z"], writes=[("qTo", hp)])
                  bfree(b)
              w_release()
              wk_, wkk = w_next(U_IN + 1)
              wkv = wk_.rearrange("p (k c) -> p k c", c=512)
              for hp in range(4):
                  b = balloc()
                  dense_T(b, lambda k, hp=hp, wkv=wkv: wkv[:, k, hp * 128:(hp + 1) * 128], 8, lambda k: hT[:, k, :], hkeys, wkk)
                  S.add("dve", lambda e, hp=hp, b=b, t0=t0: e.tensor_copy(out=kT[:, hp, t0:t0 + T], in_=bank(b)),
                        reads=bk(b), writes=[("kT", hp, ti)])
                  bfree(b)
              w_release()
              wv_, wvk = w_next(U_IN + 2)
              wvv = wv_.rearrange("p (k c) -> p k c", c=512)
              for blk in range(4):
                  b = balloc()
                  dense_T(b, lambda k, blk=blk: hT[:, k, blk * 128:(blk + 1) * 128], 8, lambda k, wvv=wvv: wvv[:, k, :],
                          lambda k: [], wvk + [("hT", k) for k in range(8)])
                  dst = v1[:, par, blk, :].rearrange("p (h c) -> p h c", c=65)[:, :, 0:64]
                  S.add("act", lambda e, b=b, dst=dst: e.activation(out=dst, in_=bank(b).rearrange("p (h c) -> p h c", c=64), func=AF.Copy),
                        reads=bk(b), writes=[("v1", par)])
                  bfree(b)
              w_release()
              S.add("sp", lambda e, s=s, t0=t0, par=par: e.dma_start(
                  out=vscr[s, t0:t0 + T, :].rearrange("(b p) c -> p b c", p=128), in_=v1[:, par, :, :]),
                  reads=[("v1", par)], writes=[("vscr", s, ti), "vsq"], dkey=("vs", 0))
              S.add("sp", lambda e, s=s, t0=t0, par=par: e.dma_start(
                  out=v4[:, par, :, :], in_=vscr[s, t0:t0 + T, :].rearrange("(a r) c -> a r c", r=4)),
                  reads=[("vscr", s, ti)], writes=[("v4", par)], dkey=("v4", par))
              S.add("sp", lambda e, s=s, t0=t0, n16=n16, j16=j16: e.dma_start(
                  out=v16[32 * j16:32 * j16 + 32, n16 % 2, :, :], in_=vscr[s, t0:t0 + T, :].rearrange("(a r) c -> a r c", r=16)),
                  reads=[("vscr", s, ti)], writes=[("v16", n16 % 2, j16)], dkey=("v16", n16 % 2, j16))
              wi_, wik = w_next(U_IN + 5)
              wiv = wi_.rearrange("p (k c) -> p k c", c=512)
              for blk in range(4):
                  b = balloc()
                  dense_T(b, lambda k, blk=blk: hT[:, k, blk * 128:(blk + 1) * 128], 8, lambda k, wiv=wiv: wiv[:, k, :],
                          lambda k: [], wik + [("hT", k) for k in range(8)])
                  S.add("dve", lambda e, b=b, blk=blk: e.tensor_copy(out=vtb[:, blk, :], in_=bank(b)),
                        reads=bk(b), writes=[("vtb", blk)])
                  bfree(b)

              w_release()
              if stop == 'A':
                  raise _Stop()
              S.tag = 't%d.%d:hgrn' % (s, ti)
              wqb_, wqbk = w_next(U_IN + 3)
              wfb_, wfbk = w_next(U_IN + 4)
              wgb_, wgbk = w_next(U_IN + 6)
              wqbv = wqb_.rearrange("p (k c) -> p k c", c=512)
              wfbv = wfb_.rearrange("p (k c) -> p k c", c=512)
              wgbv = wgb_.rearrange("p (k c) -> p k c", c=512)
              ktmk = ["ktmA", "ktmB"]
              Am, Amk = Rbf(9, 1), Rk(9, 1)
              osq, osqk = Rbf(10, 1), Rk(10, 1)

              def hgrn_head(h, T1, T1k, T2, T2k, T3, T3k, qe, qek, ke, kek, ebe_, ebek):
                  bA, bB, bC = balloc(), balloc(), balloc()
                  dense_T(bA, lambda k, h=h, wqbv=wqbv: wqbv[:, k, h * 128:(h + 1) * 128], 8, lambda k: hT[:, k, :], hkeys, wqbk)
                  dense_T(bB, lambda k, h=h, wfbv=wfbv: wfbv[:, k, h * 128:(h + 1) * 128], 8, lambda k: hT[:, k, :], hkeys, wfbk)
                  dense_T(bC, lambda k, h=h, wgbv=wgbv: wgbv[:, k, h * 128:(h + 1) * 128], 8, lambda k: hT[:, k, :], hkeys, wgbk)
                  S.add("act", lambda e, bC=bC: e.activation(out=T2, in_=bank(bC), func=AF.Sigmoid), reads=bk(bC), writes=T2k)
                  S.add("dve", lambda e, bC=bC, h=h: e.tensor_tensor(out=ybT[:, h, :], in0=bank(bC), in1=T2, op=ALU.mult),
                        reads=bk(bC) + T2k, writes=[("ybT", h)])
                  bfree(bC)
                  S.add("act", lambda e, bB=bB: e.activation(out=T1, in_=bank(bB), func=AF.Sigmoid), reads=bk(bB), writes=T1k)
                  bfree(bB)
                  S.add("dve", lambda e, h=h: e.tensor_scalar(out=T1, in0=T1, scalar1=lbc[:, 4 + h:5 + h], scalar2=lbc[:, h:h + 1],
                                                             op0=ALU.mult, op1=ALU.add), reads=T1k + ["lbc", "lbc2"], writes=T1k)
                  S.add("act", lambda e: e.activation(out=T2, in_=T1, func=AF.Ln), reads=T1k, writes=T2k)
                  S.add("dve", lambda e: e.tensor_tensor_scan(out=T3, data0=segm, data1=T2, initial=0.0, op0=ALU.mult, op1=ALU.add),
                        reads=T2k + ["cmk"], writes=T3k)
                  S.add("dve", lambda e: e.tensor_scalar(out=T1, in0=T1, scalar1=-1.0, scalar2=1.0, op0=ALU.mult, op1=ALU.add),
                        reads=T1k, writes=T1k)
                  S.add("act", lambda e: e.activation(out=T2, in_=T3, func=AF.Exp), reads=T3k, writes=T2k)
                  S.add("dve", lambda e, bA=bA: e.tensor_tensor(out=qe, in0=bank(bA), in1=T2, op=ALU.mult), reads=bk(bA) + T2k, writes=qek)
                  bfree(bA)
                  S.add("dve", lambda e: e.tensor_copy(out=ebe_, in_=T2.rearrange("p (c k) -> p c k", k=64)[:, :, 63]),
                        reads=T2k, writes=[ebek])
                  S.add("act", lambda e: e.activation(out=T2, in_=T3, func=AF.Exp, scale=-1.0), reads=T3k + [ebek], writes=T2k)
                  S.add("dve", lambda e: e.tensor_tensor(out=ke, in0=T1, in1=T2, op=ALU.mult), reads=T1k + T2k, writes=kek)
                  T3v = T3.rearrange("p (c k) -> p c k", k=64)
                  T2v = T2.rearrange("p (c k) -> p c k", k=64)
                  S.add("dve", lambda e, T3v=T3v, T2v=T2v: e.tensor_tensor(out=T2v, in0=T3v[:, :, 63:64].broadcast_to([128, 8, 64]),
                                                                           in1=T3v, op=ALU.subtract), reads=T3k, writes=T2k)
                  S.add("act", lambda e: e.activation(out=T2, in_=T2, func=AF.Exp), reads=T2k, writes=T2k)
                  S.add("dve", lambda e: e.tensor_tensor(out=T2, in0=T1, in1=T2, op=ALU.mult), reads=T1k + T2k, writes=T2k)
                  if stop == 'B1':
                      raise _Stop()
                  bD = balloc()

                  def fnT(e, bD=bD):
                      r = None
                      for blk in range(4):
                          r = e.transpose(bank(bD)[:, blk * 128:(blk + 1) * 128], T2[:, blk * 128:(blk + 1) * 128], ident)
                      return r
                  S.add("pe", fnT, reads=T2k + ["cst"], writes=bk(bD))
                  S.add("act", lambda e, bD=bD: e.activation(out=ktmA[0:64, :, :], in_=bank(bD)[0:64, :].rearrange("p (b k) -> p b k", k=128),
                                                             func=AF.Copy), reads=bk(bD) + ["ktmA_z"], writes=["ktmA"])
                  S.add("dve", lambda e, bD=bD: e.tensor_copy(out=ktmB[64:128, :, :], in_=bank(bD)[64:128, :].rearrange("p (b k) -> p b k", k=128)),
                        reads=bk(bD) + ["ktmB_z"], writes=["ktmB"])
                  bfree(bD)
                  if stop == 'B2':
                      raise _Stop()
                  bE = balloc()

                  def fnA(e, bE=bE):
                      r = None
                      for blk in range(4):
                          sl = slice(blk * 128, (blk + 1) * 128)
                          r = e.matmul(bank(bE)[:, sl], lhsT=ke[:, sl], rhs=qe[:, sl], start=True, stop=True)
                      return r
                  S.add("pe", fnA, reads=kek + qek, writes=bk(bE))
                  S.add("dve", lambda e, bE=bE: e.tensor_tensor(out=Am, in0=bank(bE), in1=cmk[:, M_HG:M_HG + 512], op=ALU.mult),
                        reads=bk(bE) + ["cmk"], writes=Amk)
                  bfree(bE)
                  if stop == 'B3':
                      raise _Stop()
                  bF, bG = balloc(), balloc()

                  def fnU(e, bF=bF, bG=bG, h=h):
                      r = None
                      for c in range(8):
                          blk, half = c // 2, c % 2
                          bb = bF if half == 0 else bG
                          r = e.matmul(bank(bb)[:, blk * 128:(blk + 1) * 128],
                                       lhsT=(ktmA if half == 0 else ktmB)[:, blk, :],
                                       rhs=vtb[:, blk, h * 128:(h + 1) * 128], start=True, stop=True)
                      return r
                  S.add("pe", fnU, reads=ktmk + [("vtb", blk) for blk in range(4)], writes=bk(bF) + bk(bG))
                  if ti == 0:
                      S.add("dve", lambda e, h=h: e.memset(St[:, h, :], 0.0), writes=[("St", h)])
                  for c in range(8):
                      S.add("act", lambda e, c=c, h=h: e.activation(out=Sbf[:, c, :], in_=St[:, h, :], func=AF.Copy),
                            reads=[("St", h)], writes=[("Sbf", c)])
                      bb = bF if c % 2 == 0 else bG
                      S.add("dve", lambda e, c=c, h=h, bb=bb: e.scalar_tensor_tensor(
                          out=St[:, h, :], in0=St[:, h, :], scalar=ebe_[:, c:c + 1],
                          in1=bank(bb)[:, (c // 2) * 128:(c // 2 + 1) * 128], op0=ALU.mult, op1=ALU.add),
                          reads=[("St", h), ebek] + bk(bb), writes=[("St", h)])
                  bfree(bF)
                  bfree(bG)
                  if stop == 'B4':
                      raise _Stop()
                  bH = balloc()

                  def fnO(e, bH=bH, h=h):
                      r = None
                      for blk in range(4):
                          sl = slice(blk * 128, (blk + 1) * 128)
                          e.matmul(bank(bH)[:, sl], lhsT=vtb[:, blk, h * 128:(h + 1) * 128], rhs=Am[:, sl], start=True, stop=False)
                          for half in range(2):
                              c = 2 * blk + half
                              cs = slice(c * 64, (c + 1) * 64)
                              r = e.matmul(bank(bH)[:, cs], lhsT=Sbf[:, c, :], rhs=qe[:, cs], start=False, stop=(half == 1))
                      return r
                  S.add("pe", fnO, reads=Amk + qek + [("vtb", blk) for blk in range(4)] + [("Sbf", c) for c in range(8)],
                        writes=bk(bH))
                  if stop == 'B5a':
                      raise _Stop()
                  S.add("act", lambda e, bH=bH: e.activation(out=osq, in_=bank(bH), func=AF.Square), reads=bk(bH), writes=osqk)
                  if stop == 'B5b':
                      raise _Stop()
                  S.add("dve", lambda e, bH=bH: e.tensor_copy(out=T1, in_=bank(bH)), reads=bk(bH), writes=T1k)
                  bfree(bH)
                  if stop == 'B5':
                      raise _Stop()
                  bI = balloc()
                  S.add("pe", lambda e, bI=bI: e.matmul(bank(bI), lhsT=onesb, rhs=osq, start=True, stop=True),
                        reads=osqk + ["cmk"], writes=bk(bI))
                  S.add("act", lambda e, bI=bI: e.activation(out=T3, in_=bank(bI), func=AF.Ln, bias=epsc, scale=1.0 / 128),
                        reads=bk(bI) + ["cst"], writes=T3k)
                  bfree(bI)
                  S.add("act", lambda e: e.activation(out=T3, in_=T3, func=AF.Exp, scale=-0.5), reads=T3k, writes=T3k)
                  S.add("dve", lambda e: e.tensor_tensor(out=T1, in0=T1, in1=T3, op=ALU.mult), reads=T1k + T3k, writes=T1k)
                  S.add("dve", lambda e, h=h: e.scalar_tensor_tensor(out=ybT[:, h, :], in0=T1, scalar=cst[:, C_HGN:C_HGN + 1],
                                                                    in1=ybT[:, h, :], op0=ALU.mult, op1=ALU.mult),
                        reads=T1k + ["cst", ("ybT", h)], writes=[("ybT", h)])

              for h in range(4):
                  if h % 2 == 0:
                      hgrn_head(h, Rf(0, 2), Rk(0, 2), Rf(2, 2), Rk(2, 2), Rf(4, 2), Rk(4, 2), Rbf(6, 1), Rk(6, 1), Rbf(7, 1), Rk(7, 1),
                                ebe[:, 0:8], "ebe0")
                  else:
                      hgrn_head(h, Rf(18, 2), Rk(18, 2), Rf(20, 2), Rk(20, 2), Rf(22, 2), Rk(22, 2), Rbf(8, 1), Rk(8, 1), Rbf(11, 1), Rk(11, 1),
                                ebe[:, 8:16], "ebe1")

              w_release(3)
              if stop == 'B':
                  raise _Stop()
              S.tag = 't%d.%d:attn' % (s, ti)
              bpool[0] = (4, 5, 6, 7)
              cur4 = cmk[:, M_CUR:M_CUR + 512]
              prev4 = cmk[:, M_PREV:M_PREV + 512]
              c16 = cmk[:, M_C16 + 512 * j16:M_C16 + 512 * j16 + 512]
              p16 = cmk[:, M_P16 + 512 * j16:M_P16 + 512 * j16 + 512]
              nsb, nsbk = Rf(12, 2), Rk(12, 2)
              dnr, dnrk = Rf(14, 2), Rk(14, 2)
              for h8 in range(8):
                  hp, e8 = h8 // 2, h8 % 2
                  pb = 64 * e8
                  bO = balloc()
                  groups = []
                  qTh = (qTe if e8 == 0 else qTo)[:, hp, :]
                  kTh = kT[:, hp, :]
                  qkey = ("qTe", hp) if e8 == 0 else ("qTo", hp)
                  g = dict(M=128, items=[], mask=cur4, mshape=(4, 128), vkeys=[("v1", par)], kkeys=[("kT", hp, ti)])
                  for i in range(4):
                      g["items"].append((kTh[:, t0 + 128 * i:t0 + 128 * i + 128], qTh[:, 128 * i:128 * i + 128], i * 128, 128,
                                         v1[:, par, i, h8 * 65:h8 * 65 + 65], bank(bO)[0:65, 128 * i:128 * i + 128]))
                  groups.append(g)
                  g = dict(M=128, items=[], mask=prev4, mshape=(4, 128), vkeys=[("v1", par), ("v1", 1 - par)],
                           kkeys=[("kT", hp, ti)] + ([("kT", hp, ti - 1)] if ti > 0 else []))
                  for i in range(4):
                      if i == 0 and ti == 0:
                          continue
                      vsrc = v1[:, par, i - 1, h8 * 65:h8 * 65 + 65] if i > 0 else v1[:, 1 - par, 3, h8 * 65:h8 * 65 + 65]
                      g["items"].append((kTh[:, t0 + 128 * (i - 1):t0 + 128 * i], qTh[:, 128 * i:128 * i + 128], i * 128, 128,
                                         vsrc, bank(bO)[0:65, 128 * i:128 * i + 128]))
                  groups.append(g)
                  g = dict(M=128, items=[], mask=cur4, mshape=(4, 128), vkeys=[("v4", par)], kkeys=[("kT", hp, ti)])
                  for r in range(4):
                      g["items"].append((kTh[:, t0 + r:t0 + T:4], qTh[:, r:T:4], r * 128, 128,
                                         v4[:, par, r, h8 * 65:h8 * 65 + 65], bank(bO)[0:65, r:T:4]))
                  groups.append(g)
                  if ti > 0:
                      g = dict(M=128, items=[], mask=prev4, mshape=(4, 128), vkeys=[("v4", 1 - par)], kkeys=[("kT", hp, ti - 1)])
                      for r in range(4):
                          g["items"].append((kTh[:, t0 - T + r:t0:4], qTh[:, r:T:4], r * 128, 128,
                                             v4[:, 1 - par, r, h8 * 65:h8 * 65 + 65], bank(bO)[0:65, r:T:4]))
                      groups.append(g)
                  M16 = 128
                  base16 = 2048 * n16
                  g = dict(M=M16, items=[], mask=c16, mshape=(16, 32), vkeys=[("v16", n16 % 2, jj) for jj in range(j16 + 1)],
                           kkeys=[("kT", hp, 4 * n16 + jj) for jj in range(j16 + 1)])
                  for r in range(16):
                      g["items"].append((kTh[:, base16 + r:base16 + 2048:16], qTh[:, r:T:16], r * 32, 32,
                                         v16[0:M16, n16 % 2, r, h8 * 65:h8 * 65 + 65], bank(bO)[0:65, r:T:16]))
                  groups.append(g)
                  if n16 > 0:
                      g = dict(M=128, items=[], mask=p16, mshape=(16, 32), vkeys=[("v16", (n16 - 1) % 2, jj) for jj in range(4)],
                               kkeys=[("kT", hp, 4 * (n16 - 1) + jj) for jj in range(4)])
                      for r in range(16):
                          g["items"].append((kTh[:, base16 - 2048 + r:base16:16], qTh[:, r:T:16], r * 32, 32,
                                             v16[:, (n16 - 1) % 2, r, h8 * 65:h8 * 65 + 65], bank(bO)[0:65, r:T:16]))
                      groups.append(g)

                  ngr = len(groups)
                  for gi, g in enumerate(groups):
                      bS = balloc()
                      M = g["M"]
                      pslot = 16 + (pt_ctr[0] % 2)
                      pt_ctr[0] += 1
                      Pt, Ptk = Rbf(pslot, 1), Rk(pslot, 1)

                      def fnS(e, g=g, bS=bS):
                          r = None
                          for (ka, qa, c0, n, va, oa) in g["items"]:
                              r = e.matmul(bank(bS)[0:g["M"], c0:c0 + n], lhsT=ka, rhs=qa, start=True, stop=True)
                          return r
                      S.add("pe", fnS, reads=g["kkeys"] + [qkey], writes=bk(bS))
                      c_lo = min(it[2] for it in g["items"])
                      c_hi = max(it[2] + it[3] for it in g["items"])
                      S.add("act", lambda e, bS=bS, M=M, Pt=Pt, c_lo=c_lo, c_hi=c_hi: e.activation(
                          out=Pt[0:M, c_lo:c_hi], in_=bank(bS)[0:M, c_lo:c_hi], func=AF.Exp, scale=0.125),
                          reads=bk(bS), writes=Ptk)
                      bfree(bS)
                      a_, b_ = g["mshape"]
                      msk = g["mask"]
                      S.add("dve", lambda e, M=M, Pt=Pt, msk=msk, c_lo=c_lo, c_hi=c_hi: e.tensor_tensor(
                          out=Pt[0:M, c_lo:c_hi], in0=Pt[0:M, c_lo:c_hi], in1=msk[0:M, c_lo:c_hi], op=ALU.mult),
                          reads=Ptk + ["cmk"], writes=Ptk)

                      def fnP(e, g=g, Pt=Pt, first_group=(gi == 0), last_group=(gi == ngr - 1)):
                          r = None
                          nit = len(g["items"])
                          for ii, (ka, qa, c0, n, va, oa) in enumerate(g["items"]):
                              r = e.matmul(oa, lhsT=va, rhs=Pt[0:g["M"], c0:c0 + n], start=(first_group and ii == 0),
                                           stop=(last_group and ii == nit - 1))
                          return r
                      S.add("pe", fnP, reads=Ptk + g["vkeys"], writes=bk(bO))
                  S.add("act", lambda e, bO=bO: e.activation(out=dnr[64:65, :], in_=bank(bO)[64:65, :], func=AF.Ln), reads=bk(bO), writes=dnrk)
                  S.add("act", lambda e, bO=bO: e.activation(out=nsb[0:64, :], in_=bank(bO)[0:64, :], func=AF.Copy), reads=bk(bO), writes=nsbk)
                  bfree(bO)
                  bZ = balloc()
                  S.add("dve", lambda e: e.tensor_copy(out=hib[64:65, :], in_=dnr[64:65, :]), reads=dnrk, writes=["hib"])
                  S.add("dve", lambda e: e.tensor_tensor(out=lob[64:65, :], in0=dnr[64:65, :], in1=hib[64:65, :], op=ALU.subtract),
                        reads=dnrk + ["hib"], writes=["lob"])

                  def fnZ(e, bZ=bZ):
                      e.matmul(bank(bZ), lhsT=cmk[:, M_SEL:M_SEL + 128], rhs=hib[:], start=True, stop=False)
                      return e.matmul(bank(bZ), lhsT=cmk[:, M_SEL:M_SEL + 128], rhs=lob[:], start=False, stop=True)
                  S.add("pe", fnZ, reads=["hib", "lob", "cmk"], writes=bk(bZ))
                  S.add("act", lambda e, bZ=bZ: e.activation(out=dnr[0:64, :], in_=bank(bZ)[0:64, :], func=AF.Exp, scale=-1.0),
                        reads=bk(bZ), writes=dnrk)
                  S.add("dve", lambda e, pb=pb, hp=hp: e.tensor_tensor(out=yaT[pb:pb + 64, hp, :], in0=nsb[0:64, :],
                                                                      in1=dnr[0:64, :], op=ALU.mult),
                        reads=dnrk + nsbk, writes=[("yaT", h8)])
                  bfree(bZ)

              if stop == 'C':
                  raise _Stop()
              S.tag = 't%d.%d:merge' % (s, ti)
              bpool[0] = None
              mT, mTk = Rbf(0, 8).rearrange("p (c t) -> p c t", t=T), (lambda c: Rk(c, 1))
              Ta, Tak = Rf(8, 2), Rk(8, 2)
              Tb, Tbk = Rf(10, 2), Rk(10, 2)
              for c in range(8):
                  wm_, wmk = w_next(U_MG + c)
                  wa = wm_[:, 0:512].rearrange("p (k m) -> p k m", m=128)
                  wb = wm_[:, 512:1024].rearrange("p (k m) -> p k m", m=128)
                  wga = wm_[:, 1024:2048].rearrange("p (k m) -> p k m", m=128)
                  wgb = wm_[:, 2048:3072].rearrange("p (k m) -> p k m", m=128)
                  b1, b2, b3, b4 = balloc(), balloc(), balloc(), balloc()
                  dense_T(b1, lambda k, wa=wa: wa[:, k, :], 4, lambda k: yaT[:, k, :], lambda k: [("yaT", 2 * k), ("yaT", 2 * k + 1)], wmk)
                  dense_T(b2, lambda k, wga=wga: wga[:, k, :], 8, lambda k: hT[:, k, :], hkeys, wmk)
                  dense_T(b3, lambda k, wb=wb: wb[:, k, :], 4, lambda k: ybT[:, k, :], lambda k: [("ybT", k)], wmk)
                  dense_T(b4, lambda k, wgb=wgb: wgb[:, k, :], 8, lambda k: hT[:, k, :], hkeys, wmk)
                  S.add("act", lambda e, b2=b2: e.activation(out=Ta, in_=bank(b2), func=AF.Sigmoid), reads=bk(b2), writes=Tak)
                  S.add("act", lambda e, b4=b4: e.activation(out=Tb, in_=bank(b4), func=AF.Sigmoid), reads=bk(b4), writes=Tbk)
                  S.add("dve", lambda e, b1=b1: e.tensor_tensor(out=Ta, in0=bank(b1), in1=Ta, op=ALU.mult), reads=bk(b1) + Tak, writes=Tak)
                  S.add("dve", lambda e, b3=b3: e.tensor_tensor(out=Tb, in0=bank(b3), in1=Tb, op=ALU.mult), reads=bk(b3) + Tbk, writes=Tbk)
                  S.add("dve", lambda e, c=c, mT=mT: e.tensor_tensor(out=mT[:, c, :], in0=Ta, in1=Tb, op=ALU.add),
                        reads=Tak + Tbk, writes=mTk(c))
                  for b in (b1, b2, b3, b4):
                      bfree(b)
                  w_release()
              for u in range(2):
                  wo_, wok = w_next(U_OUT + u)
                  wov = wo_.rearrange("p (k c) -> p k c", c=512)
                  for cc in range(4):
                      c = 4 * u + cc
                      b = balloc()
                      dense_T(b, lambda k, cc=cc, wov=wov: wov[:, k, cc * 128:(cc + 1) * 128], 8, lambda k, mT=mT: mT[:, k, :],
                              lambda k: Rk(k, 1), wok)
                      S.add("dve", lambda e, c=c, b=b: e.tensor_tensor(out=xT[:, c, :], in0=bank(b), in1=xT[:, c, :], op=ALU.add),
                            reads=bk(b) + [("xT", c)], writes=[("xT", c)])
                      bfree(b)
                  w_release()

              if stop == 'D':
                  raise _Stop()
              S.tag = 't%d.%d:ffn' % (s, ti)
              rmsnorm(1, lambda c: hT[:, c, :], lambda c: [("hT", c)])
              hid = Rbf(0, 22).rearrange("p (c t) -> p c t", t=T)
              for i2 in range(11):
                  wg_, wgk = w_next(U_GU + i2)
                  wgv = wg_.rearrange("p (k g c) -> p k g c", g=2, c=256)
                  for ih in range(2):
                      i = 2 * i2 + ih
                      bg, bu = balloc(), balloc()
                      dense_T(bg, lambda k, ih=ih, wgv=wgv: wgv[:, k, 0, ih * 128:(ih + 1) * 128], 8, lambda k: hT[:, k, :], hkeys, wgk, split=(i == 0))
                      dense_T(bu, lambda k, ih=ih, wgv=wgv: wgv[:, k, 1, ih * 128:(ih + 1) * 128], 8, lambda k: hT[:, k, :], hkeys, wgk)
                      Tg, Tgk = Rf(22, 2), Rk(22, 2)
                      S.add("act", lambda e, bg=bg, Tg=Tg: e.activation(out=Tg, in_=bank(bg), func=AF.Silu), reads=bk(bg), writes=Tgk)
                      S.add("dve", lambda e, bu=bu, i=i, hid=hid, Tg=Tg: e.tensor_tensor(out=hid[:, i, :], in0=bank(bu), in1=Tg, op=ALU.mult),
                            reads=bk(bu) + Tgk, writes=Rk(i, 1))
                      bfree(bg)
                      bfree(bu)
                  w_release()
              for c in range(8):
                  wd_, wdk = w_next(U_DN + c)
                  wdv = wd_[:, 0:2816].rearrange("p (k m) -> p k m", m=128)
                  b = balloc()
                  dense_T(b, lambda k, wdv=wdv: wdv[:, k, :], 22, lambda k, hid=hid: hid[:, k, :], lambda k: Rk(k, 1), wdk)
                  S.add("dve", lambda e, c=c, b=b: e.tensor_tensor(out=xT[:, c, :], in0=bank(b), in1=xT[:, c, :], op=ALU.add),
                        reads=bk(b) + [("xT", c)], writes=[("xT", c)])
                  bfree(b)
                  w_release()

              if stop == 'E':
                  raise _Stop()
              S.tag = 't%d.%d:ple' % (s, ti)
              rmsnorm(2, lambda c: hT[:, c, :], lambda c: [("hT", c)])
              Tp, Tpk = Rf(0, 2), Rk(0, 2)
              pgu = []
              for u in range(2):
                  pgu.append(w_next(U_PG + u))
              wpe_, wpek = w_next(U_PE)
              wpev = wpe_[:, 0:2048].rearrange("p (k c) -> p k c", c=1024)
              for c in range(8):
                  wpg_, wpgk = pgu[c // 4]
                  wpgv = wpg_.rearrange("p (k c) -> p k c", c=512)
                  cc = c % 4
                  bp, be_ = balloc(), balloc()
                  dense_T(bp, lambda k, cc=cc, wpgv=wpgv: wpgv[:, k, cc * 128:(cc + 1) * 128], 8, lambda k: hT[:, k, :], hkeys, wpgk, split=(c == 0))
                  dense_T(be_, lambda k, c=c, wpev=wpev: wpev[:, k, c * 128:(c + 1) * 128], 2, lambda k: pT[:, k, :], lambda k: [("pT", k)], wpek)
                  S.add("act", lambda e, bp=bp: e.activation(out=Tp, in_=bank(bp), func=AF.Sigmoid), reads=bk(bp), writes=Tpk)
                  S.add("dve", lambda e, be_=be_: e.tensor_tensor(out=Tp, in0=bank(be_), in1=Tp, op=ALU.mult), reads=bk(be_) + Tpk, writes=Tpk)
                  S.add("dve", lambda e, c=c: e.tensor_tensor(out=xT[:, c, :], in0=xT[:, c, :], in1=Tp, op=ALU.add),
                        reads=Tpk + [("xT", c)], writes=[("xT", c)])
                  bfree(bp)
                  bfree(be_)
              w_release(3)

              if stop == 'F':
                  raise _Stop()
              S.tag = 't%d.%d:final' % (s, ti)
              rmsnorm(3, lambda c: xT[:, c, :], lambda c: [("xT", c)])
              for blk in range(4):
                  osl = 4 + 4 * (blk % 2)
                  otm, otmk = Rf(osl, 4), Rk(osl, 4)
                  for half in range(2):
                      b = balloc()

                      def fnF(e, b=b, blk=blk, half=half):
                          r = None
                          for cc in range(4):
                              r = e.transpose(bank(b)[:, cc * 128:(cc + 1) * 128], xT[:, half * 4 + cc, blk * 128:(blk + 1) * 128], ident)
                          return r
                      S.add("pe", fnF, reads=[("xT", half * 4 + cc) for cc in range(4)] + ["cst"], writes=bk(b))
                      if half == 0:
                          S.add("act", lambda e, b=b, otm=otm: e.activation(out=otm[:, 0:512], in_=bank(b), func=AF.Copy),
                                reads=bk(b), writes=Rk(osl, 2))
                      else:
                          S.add("dve", lambda e, b=b, otm=otm: e.tensor_copy(out=otm[:, 512:1024], in_=bank(b)),
                                reads=bk(b), writes=Rk(osl + 2, 2))
                      bfree(b)
                  S.add("sp", lambda e, otm=otm, blk=blk, row0=row0: e.dma_start(out=out_d[row0 + 128 * blk:row0 + 128 * (blk + 1), :], in_=otm),
                        reads=otmk, writes=[("outd", s, ti, blk)], dkey=("out", blk % 2))

    except _Stop:
        pass
    assert stop is not None or wstate["consumed"] == len(unit_seq)
    S.emit(nc, stack)
    stack.close()
    return nc, S


def _pack_weights(w_in, w_a_up, w_b_up, w_out, w_gu, w_down, w_pe, w_pg):
    wp = np.zeros((NU, 128, USZ), np.float32)

    def kc(w, c0, c1):
        nk = w.shape[0] // 128
        return w[:, c0:c1].reshape(nk, 128, c1 - c0).transpose(1, 0, 2)

    for u in range(11):
        wp[U_IN + u] = kc(w_in, 512 * u, 512 * u + 512).reshape(128, -1)
    for c in range(8):
        wp[U_MG + c, :, 0:512] = kc(w_a_up, 128 * c, 128 * c + 128).reshape(128, -1)
        wp[U_MG + c, :, 512:1024] = kc(w_b_up, 128 * c, 128 * c + 128).reshape(128, -1)
        wp[U_MG + c, :, 1024:2048] = kc(w_in, 3584 + 128 * c, 3584 + 128 * c + 128).reshape(128, -1)
        wp[U_MG + c, :, 2048:3072] = kc(w_in, 4608 + 128 * c, 4608 + 128 * c + 128).reshape(128, -1)
    for u in range(2):
        wp[U_OUT + u] = kc(w_out, 512 * u, 512 * u + 512).reshape(128, -1)
        wp[U_PG + u] = kc(w_pg, 512 * u, 512 * u + 512).reshape(128, -1)
    for i2 in range(11):
        g = kc(w_gu, 256 * i2, 256 * i2 + 256)
        uu = kc(w_gu, FFN + 256 * i2, FFN + 256 * i2 + 256)
        wp[U_GU + i2] = np.stack([g, uu], axis=2).reshape(128, -1)
    for c in range(8):
        wp[U_DN + c, :, 0:2816] = kc(w_down, 128 * c, 128 * c + 128).reshape(128, -1)
    wp[U_PE, :, 0:2048] = kc(w_pe, 0, 1024).reshape(128, -1)
    return wp


def _consts(norm_mix, norm_ffn, norm_ple, norm_final, hg_norm, hg_lb):
    cst = np.zeros((128, NCF), np.float32)
    cst[:, C_ID:C_ID + 128] = np.eye(128, dtype=np.float32)
    for gi, g in enumerate((norm_mix, norm_ffn, norm_ple, norm_final)):
        cst[:, C_G + 8 * gi:C_G + 8 * gi + 8] = np.asarray(g, np.float32).reshape(8, 128).T
    cst[:, C_HGN] = np.asarray(hg_norm, np.float32).reshape(128)
    cst[:, C_LB:C_LB + 8] = np.asarray(hg_lb, np.float32).reshape(2, 4, 128).transpose(2, 0, 1).reshape(128, 8)
    cst[:, C_EPS] = EPS
    mk = np.zeros((128, NMK), np.float32)
    kk = np.arange(128)[:, None]
    qq = np.arange(128)[None, :]
    mk[:, M_CUR:M_CUR + 512] = np.tile((kk <= qq), (1, 4))
    mk[:, M_PREV:M_PREV + 512] = np.tile((kk >= qq), (1, 4))
    mk[:, M_HG:M_HG + 512] = np.tile((kk <= qq) & ((kk // 64) == (qq // 64)), (1, 4))
    a2 = np.arange(32)[None, :]
    for j in range(4):
        mk[:, M_C16 + 512 * j:M_C16 + 512 * j + 512] = np.tile((kk <= 32 * j + a2), (1, 16))
        mk[:, M_P16 + 512 * j:M_P16 + 512 * j + 512] = np.tile((kk >= 32 * j + a2), (1, 16))
    mk[:, M_ONE:M_ONE + 128] = 1.0
    mk[64, M_SEL:M_SEL + 128] = 1.0
    seg = np.ones(512, np.float32)
    seg[0::64] = 0.0
    mk[:, M_SEG:M_SEG + 512] = seg[None, :]
    return cst, mk


_CACHE = {}


def kernel(x, p, norm_mix, w_in, hg_lb, hg_norm, w_a_up, w_b_up, w_out, norm_ffn,
           w_gu, w_down, norm_ple, w_pe, w_pg, norm_final):
    f = lambda a: np.ascontiguousarray(np.asarray(a, dtype=np.float32))
    x = f(x)
    p = f(p)
    wp = _pack_weights(f(w_in)[0], f(w_a_up)[0], f(w_b_up)[0], f(w_out)[0], f(w_gu)[0], f(w_down)[0], f(w_pe)[0], f(w_pg)[0])
    cst, mk = _consts(f(norm_mix)[0], f(norm_ffn)[0], f(norm_ple)[0], f(norm_final), f(hg_norm)[0], f(hg_lb))
    if "nc" not in _CACHE:
        _CACHE["nc"] = build_program()[0]
    nc = _CACHE["nc"]
    in_maps = []
    for c in range(NCORES):
        in_maps.append({
            "x": x[NSEQ * c:NSEQ * (c + 1)].reshape(NSEQ * SEQ, D),
            "p": p[0, NSEQ * c:NSEQ * (c + 1)].reshape(NSEQ * SEQ, 256),
            "wpack": wp, "cst": cst, "cmask": mk,
        })
    res = run_bass_kernel_spmd(nc, in_maps, core_ids=list(range(NCORES)))
    out = np.concatenate([np.asarray(r["out"]).reshape(NSEQ, SEQ, D) for r in res.results], axis=0)
    return out.astype(np.float32)
```
